# Optimizing a Trainium2 kernel written in Bass

```python
import jax, jax.numpy as jnp
from jax import lax
import numpy as np

D_MODEL = 2048
BATCH = 8
SEQ = 2048
DEPTH = 2

GRID_W = 64
CTX_LEN = 256
EPS = 1e-6
NEG_INF = -1e30
ROPE_THETA = 10000.0

A_WIDTH = 1024
A_CHUNK = 128
A_GROUPS = 8
A_GDIM = A_WIDTH // A_GROUPS

NA_HEADS = 8
NA_HDIM = 128
NA_WIDTH = NA_HEADS * NA_HDIM
NA_KH = 8
NA_KW = 16
NA_QB = 16
NA_KSPAN = 32
NA_NCB = GRID_W // NA_QB

M_HEADS = 4
M_DK = 128
M_DV = 256
M_QK_W = M_HEADS * M_DK
M_V_W = M_HEADS * M_DV
M_CHUNK = 128
M_NGATE = 4 * M_HEADS
M_F_BIAS = 3.0

N_BRANCH = 3
BRANCH_W = 1024

N_EXPERTS = 16
N_GROUPS = 4
EXP_PER_GROUP = N_EXPERTS // N_GROUPS
TOP_K = 2
D_FF_EXPERT = 1024

FIELDS = (('a_u', A_WIDTH), ('a_v', A_WIDTH), ('b_q', NA_WIDTH), ('b_k', NA_WIDTH), ('b_v', NA_WIDTH), ('c_q', M_QK_W), ('c_k', M_QK_W), ('c_v', M_V_W), ('c_o', M_V_W), ('c_g', M_NGATE), ('gate', N_BRANCH * D_MODEL))
ALL_FIELDS = ('a_u', 'a_v', 'b_q', 'b_k', 'b_v', 'c_q', 'c_k', 'c_v', 'c_o', 'c_g', 'gate')
CTX_FIELDS = ('b_k', 'b_v', 'c_k', 'c_v', 'c_g')
P_IN = 2 * A_WIDTH + 3 * NA_WIDTH + 2 * M_QK_W + 2 * M_V_W + M_NGATE + N_BRANCH * D_MODEL

kernel_name = 'hybrid_dit_natten_mlstm_sgu_grouped_moe'


def _rmsnorm(x, g):
    x32 = x.astype(jnp.float32)
    y = x32 * lax.rsqrt(jnp.mean(x32 * x32, axis=-1, keepdims=True) + EPS)
    return (y * g.astype(jnp.float32)).astype(x.dtype)


def _modulate(x, g, shift, scale):
    return _rmsnorm(x, g) * (1 + scale) + shift


def _project(h, w_in, names):
    offs, o = {}, 0
    for name, width in FIELDS:
        offs[name] = (o, width)
        o += width
    if len(names) == len(FIELDS):
        w = w_in
    else:
        w = jnp.concatenate([w_in[:, offs[n][0]:offs[n][0] + offs[n][1]] for n in names], axis=1)
    p = h @ w
    out, o = {}, 0
    for n in names:
        out[n] = p[..., o:o + offs[n][1]]
        o += offs[n][1]
    return out


def _heads(t, nh):
    return t.reshape(t.shape[:-1] + (nh, -1))


def _bhtd(t, nh):
    b, tl, _ = t.shape
    return t.reshape(b, tl, nh, -1).transpose(0, 2, 1, 3)


def _rope_1d(x, pos):
    d = x.shape[-1]
    half = d // 2
    inv = ROPE_THETA ** (-jnp.arange(half, dtype=jnp.float32) / half)
    ang = pos.astype(jnp.float32)[:, None] * inv[None, :]
    cos, sin = jnp.cos(ang), jnp.sin(ang)
    x32 = x.astype(jnp.float32)
    x1, x2 = x32[..., :half], x32[..., half:]
    return jnp.concatenate([x1 * cos - x2 * sin, x1 * sin + x2 * cos], axis=-1).astype(x.dtype)


def _rope_2d(x, pos_row, pos_col):
    half = x.shape[-1] // 2
    return jnp.concatenate([_rope_1d(x[..., :half], pos_row), _rope_1d(x[..., half:], pos_col)], axis=-1)


def _sgu(u_pre, v_pre, g_sgu, w_sp, b_sp):
    bsz, tl, _ = v_pre.shape
    u = jax.nn.gelu(u_pre)
    v = _rmsnorm(jax.nn.gelu(v_pre), g_sgu)
    vg = v.reshape(bsz, tl // A_CHUNK, A_CHUNK, A_GROUPS, A_GDIM)
    mixed = jnp.einsum('gij,bnjgc->bnigc', w_sp, vg) + b_sp.T[None, None, :, :, None]
    return u * mixed.reshape(bsz, tl, A_WIDTH)


def _na_col_tables():
    cq = np.arange(GRID_W)
    cs = np.clip(cq - NA_KW // 2, 0, GRID_W - NA_KW)
    ks = np.clip(np.arange(NA_NCB) * NA_QB - NA_KW // 2, 0, GRID_W - NA_KSPAN)
    key_cols = ks[:, None] + np.arange(NA_KSPAN)[None, :]
    kc = key_cols[cq // NA_QB]
    valid = (kc >= cs[:, None]) & (kc < cs[:, None] + NA_KW)
    off = np.clip(kc - cq[:, None], -(NA_KW - 1), NA_KW - 1) + NA_KW - 1
    return key_cols, valid.reshape(NA_NCB, NA_QB, NA_KSPAN), off.reshape(NA_NCB, NA_QB, NA_KSPAN)


def _neighbourhood_attention(q, k, v, k_ctx, v_ctx, rpb, rows):
    bsz = q.shape[0]
    kh = min(NA_KH, rows)
    key_cols, valid, col_off = _na_col_tables()
    scale = NA_HDIM ** -0.5
    qg = q.reshape(bsz, rows, NA_NCB, NA_QB, NA_HEADS, NA_HDIM)
    kg = k.reshape(bsz, rows, GRID_W, NA_HEADS, NA_HDIM)
    vg = v.reshape(bsz, rows, GRID_W, NA_HEADS, NA_HDIM)
    rpb_cols = rpb[:, :, col_off].astype(jnp.float32)
    n_win = kh * NA_KSPAN

    def row_block(r):
        rs = jnp.clip(r - kh // 2, 0, rows - kh)
        q_r = lax.dynamic_index_in_dim(qg, r, axis=1, keepdims=False)
        k_r = lax.dynamic_slice_in_dim(kg, rs, kh, axis=1)[:, :, key_cols]
        v_r = lax.dynamic_slice_in_dim(vg, rs, kh, axis=1)[:, :, key_cols]
        s_win = jnp.einsum('bjqhd,bajkhd->bhjqak', q_r, k_r).astype(jnp.float32) * scale
        row_idx = rs + jnp.arange(kh) - r + NA_KH - 1
        bias = jnp.take(rpb_cols, row_idx, axis=1).transpose(0, 2, 3, 1, 4)
        s_win = jnp.where(valid[:, :, None, :], s_win + bias[None], NEG_INF)
        s_ctx = jnp.einsum('bjqhd,bshd->bhjqs', q_r, k_ctx).astype(jnp.float32) * scale
        s = jnp.concatenate([s_win.reshape(s_win.shape[:4] + (n_win,)), s_ctx], axis=-1)
        p = jax.nn.softmax(s, axis=-1).astype(v.dtype)
        p_win = p[..., :n_win].reshape(s_win.shape)
        o = jnp.einsum('bhjqak,bajkhd->bjqhd', p_win, v_r) + jnp.einsum('bhjqs,bshd->bjqhd', p[..., n_win:], v_ctx)
        return o.reshape(bsz, GRID_W, NA_WIDTH)

    out = lax.map(row_block, jnp.arange(rows))
    return jnp.moveaxis(out, 0, 1).reshape(bsz, rows * GRID_W, NA_WIDTH)


def _ctx_attention(q, k, v):
    bsz, s_len = q.shape[:2]
    s = jnp.einsum('bqhd,bkhd->bhqk', q, k).astype(jnp.float32) * (NA_HDIM ** -0.5)
    p = jax.nn.softmax(s, axis=-1).astype(v.dtype)
    return jnp.einsum('bhqk,bkhd->bqhd', p, v).reshape(bsz, s_len, NA_WIDTH)


def _zero_state(bsz):
    return (jnp.zeros((bsz, M_HEADS, M_DK, M_DV), jnp.float32), jnp.zeros((bsz, M_HEADS, M_DK), jnp.float32), jnp.zeros((bsz, M_HEADS), jnp.float32))


def _flip(a):
    return jnp.flip(a, axis=2)


def _mlstm_chunkwise(q, k, v, i_pre, f_pre, state):
    bsz, nh, tl = k.shape[:3]
    nc = tl // M_CHUNK

    def chunks(a):
        a = a.astype(jnp.float32).reshape((bsz, nh, nc, M_CHUNK) + a.shape[3:])
        return jnp.moveaxis(a, 2, 0)

    log_f = jax.nn.log_sigmoid(f_pre.astype(jnp.float32))
    with_out = q is not None
    xs = (chunks(k), chunks(v), chunks(i_pre), chunks(log_f))
    if with_out:
        xs = xs + (chunks(q),)
    order = jnp.tril(jnp.ones((M_CHUNK, M_CHUNK), dtype=bool))

    def step(carry, xs_c):
        c_st, n_st, m_st = carry
        kc, vc, ic, lfc = xs_c[0], xs_c[1], xs_c[2], xs_c[3]
        b = jnp.cumsum(lfc, axis=-1)
        b_last = b[..., -1]
        w_log = b_last[..., None] - b + ic
        m_new = jnp.maximum(b_last + m_st, jnp.max(w_log, axis=-1))
        decay = jnp.exp(b_last + m_st - m_new)
        kw = kc * jnp.exp(w_log - m_new[..., None])[..., None]
        c_new = decay[..., None, None] * c_st + jnp.einsum('bhsk,bhsv->bhkv', kw, vc)
        n_new = decay[..., None] * n_st + kw.sum(axis=2)
        if not with_out:
            return (c_new, n_new, m_new), None
        qc = xs_c[4]
        d_log = jnp.where(order, b[..., :, None] - b[..., None, :] + ic[..., None, :], -jnp.inf)
        inter = b + m_st[..., None]
        m_row = jnp.maximum(inter, jnp.max(d_log, axis=-1))
        s = jnp.einsum('bhtk,bhsk->bhts', qc, kc) * jnp.exp(d_log - m_row[..., None])
        ew = jnp.exp(inter - m_row)
        num = jnp.einsum('bhts,bhsv->bhtv', s, vc) + ew[..., None] * jnp.einsum('bhtk,bhkv->bhtv', qc, c_st)
        den = s.sum(axis=-1) + ew * jnp.einsum('bhtk,bhk->bht', qc, n_st)
        h = num / jnp.maximum(jnp.abs(den), jnp.exp(-m_row))[..., None]
        return (c_new, n_new, m_new), h

    carry, h = lax.scan(step, state, xs)
    if not with_out:
        return carry
    return jnp.moveaxis(h, 0, 2).reshape(bsz, nh, tl, M_DV), carry


def _mlstm_gates(g_pre, b_mgate):
    bsz, tl, _ = g_pre.shape
    g = g_pre.reshape(bsz, tl, 4, M_HEADS).astype(jnp.float32) + b_mgate.astype(jnp.float32)
    return jnp.transpose(g, (2, 0, 3, 1))


def _mlstm_out(h, o_pre, g_mnorm):
    bsz, nh, tl, dv = h.shape
    y = _rmsnorm(h, g_mnorm.reshape(nh, 1, dv))
    y = y.transpose(0, 2, 1, 3).reshape(bsz, tl, nh * dv).astype(o_pre.dtype)
    return y * jax.nn.sigmoid(o_pre)


def _merge(gate_pre, branches, w_branch, w_out):
    g = jax.nn.sigmoid(gate_pre.reshape(gate_pre.shape[:-1] + (N_BRANCH, D_MODEL)))
    merged = g[..., 0, :] * (branches[0] @ w_branch[0])
    for i in range(1, N_BRANCH):
        merged = merged + g[..., i, :] * (branches[i] @ w_branch[i])
    return merged @ w_out


def _moe(h, w_router, b_router, w_eg, w_eu, w_ed):
    n_tok = h.shape[0]
    scores = jax.nn.sigmoid((h @ w_router).astype(jnp.float32))
    sel = (scores + b_router.astype(jnp.float32)).reshape(n_tok, N_GROUPS, EXP_PER_GROUP)
    group_score = lax.top_k(sel, TOP_K)[0].sum(axis=-1)
    g_idx = jnp.argmax(group_score, axis=-1)
    sel_in = jnp.take_along_axis(sel, g_idx[:, None, None], axis=1)[:, 0]
    _, e_local = lax.top_k(sel_in, TOP_K)
    e_idx = g_idx[:, None] * EXP_PER_GROUP + e_local
    w_sel = jnp.take_along_axis(scores, e_idx, axis=1)
    w_sel = w_sel / jnp.sum(w_sel, axis=-1, keepdims=True)
    combine = jnp.sum(jax.nn.one_hot(e_idx, N_EXPERTS, dtype=jnp.float32) * w_sel[..., None], axis=1).astype(h.dtype)
    out = jnp.zeros_like(h)
    for e in range(N_EXPERTS):
        he = jax.nn.silu(h @ w_eg[e]) * (h @ w_eu[e])
        out = out + combine[:, e:e + 1] * (he @ w_ed[e])
    return out


def _layer(xl, xc, c, c_ctx, w_ada, b_ada, g1, g2, w_in, g_sgu, w_sp, b_sp, rpb, b_mgate, g_mnorm, w_branch, w_out, w_router, b_router, w_eg, w_eu, w_ed, last):
    bsz, seq, d = xl.shape
    ctx_len = xc.shape[1]
    rows = seq // GRID_W
    t = jnp.arange(seq)
    pos_row, pos_col = t // GRID_W, t % GRID_W

    mod = jax.nn.silu(c) @ w_ada + b_ada
    mod_c = jax.nn.silu(c_ctx) @ w_ada + b_ada
    sh1, sc1, gt1, sh2, sc2, gt2 = [m[:, None, :] for m in jnp.split(mod, 6, axis=-1)]
    csh1, csc1, cgt1, csh2, csc2, cgt2 = jnp.split(mod_c, 6, axis=-1)

    pl = _project(_modulate(xl, g1, sh1, sc1), w_in, ALL_FIELDS)
    pc = _project(_modulate(xc, g1, csh1, csc1), w_in, CTX_FIELDS if last else ALL_FIELDS)

    ya = _sgu(pl['a_u'], pl['a_v'], g_sgu, w_sp, b_sp)

    k_bc, v_bc = _heads(pc['b_k'], NA_HEADS), _heads(pc['b_v'], NA_HEADS)
    yb = _neighbourhood_attention(_heads(pl['b_q'], NA_HEADS), _heads(pl['b_k'], NA_HEADS), _heads(pl['b_v'], NA_HEADS), k_bc, v_bc, rpb, rows)

    k_cc, v_cc = _bhtd(pc['c_k'], M_HEADS), _bhtd(pc['c_v'], M_HEADS)
    g_cc = _mlstm_gates(pc['c_g'], b_mgate)
    state0 = _zero_state(bsz)
    if last:
        st_f = _mlstm_chunkwise(None, k_cc, v_cc, g_cc[0], g_cc[1], state0)
        st_b = _mlstm_chunkwise(None, _flip(k_cc), _flip(v_cc), _flip(g_cc[2]), _flip(g_cc[3]), state0)
    else:
        q_cc = _bhtd(pc['c_q'], M_HEADS) * (M_DK ** -0.5)
        hc_f, st_f = _mlstm_chunkwise(q_cc, k_cc, v_cc, g_cc[0], g_cc[1], state0)
        hc_b, st_b = _mlstm_chunkwise(_flip(q_cc), _flip(k_cc), _flip(v_cc), _flip(g_cc[2]), _flip(g_cc[3]), state0)
        yc_ctx = _mlstm_out(hc_f + _flip(hc_b), pc['c_o'], g_mnorm)
    q_m = _rope_2d(_bhtd(pl['c_q'], M_HEADS), pos_row, pos_col) * (M_DK ** -0.5)
    k_m = _rope_2d(_bhtd(pl['c_k'], M_HEADS), pos_row, pos_col)
    v_m = _bhtd(pl['c_v'], M_HEADS)
    g_m = _mlstm_gates(pl['c_g'], b_mgate)
    h_f, _ = _mlstm_chunkwise(q_m, k_m, v_m, g_m[0], g_m[1], st_f)
    h_b, _ = _mlstm_chunkwise(_flip(q_m), _flip(k_m), _flip(v_m), _flip(g_m[2]), _flip(g_m[3]), st_b)
    yc = _mlstm_out(h_f + _flip(h_b), pl['c_o'], g_mnorm)

    xl = xl + gt1 * _merge(pl['gate'], (ya, yb, yc), w_branch, w_out)
    h2l = _modulate(xl, g2, sh2, sc2)
    if last:
        f = _moe(h2l.reshape(-1, d), w_router, b_router, w_eg, w_eu, w_ed)
        return xl + gt2 * f.reshape(bsz, seq, d), None

    ya_ctx = _sgu(pc['a_u'], pc['a_v'], g_sgu, w_sp, b_sp)
    yb_ctx = _ctx_attention(_heads(pc['b_q'], NA_HEADS), k_bc, v_bc)
    xc = xc + cgt1 * _merge(pc['gate'], (ya_ctx, yb_ctx, yc_ctx), w_branch, w_out)
    h2c = _modulate(xc, g2, csh2, csc2)
    f = _moe(jnp.concatenate([h2l.reshape(-1, d), h2c.reshape(-1, d)], axis=0), w_router, b_router, w_eg, w_eu, w_ed)
    n_lat = bsz * seq
    xl = xl + gt2 * f[:n_lat].reshape(bsz, seq, d)
    xc = xc + cgt2 * f[n_lat:].reshape(bsz, ctx_len, d)
    return xl, xc


def setup_inputs(seed: int = 0) -> dict:
    key = jax.random.key(seed)
    ks = jax.random.split(key, 24)
    f32 = jnp.float32

    def nrm(k, shape, scale):
        return jax.random.normal(k, shape, f32) * scale

    def gain(k, shape):
        return 1.0 + 0.02 * jax.random.normal(k, shape, f32)

    gate_offset = jnp.array([0.0, M_F_BIAS, 0.0, M_F_BIAS], f32)[:, None]
    return {
        'x': nrm(ks[0], (BATCH, SEQ, D_MODEL), 1.0),
        'c': nrm(ks[1], (BATCH, D_MODEL), 1.0),
        'ctx': nrm(ks[2], (BATCH, CTX_LEN, D_MODEL), 1.0),
        'c_ctx': nrm(ks[3], (D_MODEL,), 1.0),
        'w_ada': nrm(ks[4], (DEPTH, D_MODEL, 6 * D_MODEL), 0.5 * D_MODEL ** -0.5),
        'b_ada': nrm(ks[5], (DEPTH, 6 * D_MODEL), 0.01),
        'g_norm1': gain(ks[6], (DEPTH, D_MODEL)),
        'g_norm2': gain(ks[7], (DEPTH, D_MODEL)),
        'w_in': nrm(ks[8], (DEPTH, D_MODEL, P_IN), D_MODEL ** -0.5),
        'g_sgu': gain(ks[9], (DEPTH, A_WIDTH)),
        'w_spatial': nrm(ks[10], (DEPTH, A_GROUPS, A_CHUNK, A_CHUNK), A_CHUNK ** -0.5),
        'b_spatial': gain(ks[11], (DEPTH, A_GROUPS, A_CHUNK)),
        'na_rpb': nrm(ks[12], (DEPTH, NA_HEADS, 2 * NA_KH - 1, 2 * NA_KW - 1), 0.1),
        'b_mgate': gate_offset + nrm(ks[13], (DEPTH, 4, M_HEADS), 0.1),
        'g_mnorm': gain(ks[14], (DEPTH, M_V_W)),
        'w_branch': nrm(ks[15], (DEPTH, N_BRANCH, BRANCH_W, D_MODEL), BRANCH_W ** -0.5),
        'w_out': nrm(ks[16], (DEPTH, D_MODEL, D_MODEL), D_MODEL ** -0.5),
        'w_router': nrm(ks[17], (D_MODEL, N_EXPERTS), D_MODEL ** -0.5),
        'b_router': nrm(ks[18], (N_EXPERTS,), 0.01),
        'w_e_gate': nrm(ks[19], (DEPTH, N_EXPERTS, D_MODEL, D_FF_EXPERT), D_MODEL ** -0.5),
        'w_e_up': nrm(ks[20], (DEPTH, N_EXPERTS, D_MODEL, D_FF_EXPERT), D_MODEL ** -0.5),
        'w_e_down': nrm(ks[21], (DEPTH, N_EXPERTS, D_FF_EXPERT, D_MODEL), D_FF_EXPERT ** -0.5),
        'g_final': gain(ks[22], (D_MODEL,)),
    }


def reference(x, c, ctx, c_ctx, w_ada, b_ada, g_norm1, g_norm2, w_in, g_sgu, w_spatial, b_spatial, na_rpb, b_mgate, g_mnorm, w_branch, w_out, w_router, b_router, w_e_gate, w_e_up, w_e_down, g_final):
    xl, xc = x, ctx
    for layer in range(DEPTH):
        xl, xc = _layer(xl, xc, c, c_ctx, w_ada[layer], b_ada[layer], g_norm1[layer], g_norm2[layer], w_in[layer], g_sgu[layer], w_spatial[layer], b_spatial[layer], na_rpb[layer], b_mgate[layer], g_mnorm[layer], w_branch[layer], w_out[layer], w_router, b_router, w_e_gate[layer], w_e_up[layer], w_e_down[layer], last=(layer == DEPTH - 1))
    return _rmsnorm(xl, g_final)
```

```python
import contextlib
import numpy as np
import ml_dtypes
import concourse.bass as bass
import concourse.mybir as mybir
from concourse.bass_utils import run_bass_kernel_spmd

F32 = mybir.dt.float32
BF16 = mybir.dt.bfloat16
AF = mybir.ActivationFunctionType
ALU = mybir.AluOpType

D = 2048
SEQ = 2048
CTX = 256
NT = SEQ + CTX
NTILE = NT // 128
L = 2
EPS = 1e-6
NE = 16
GELU_C = 1.5957691216057308
NA_SCALE = 128 ** -0.5


_UN = [0]


def un(name):
    _UN[0] += 1
    return "t%d_%s" % (_UN[0], name)


class Sem:
    __slots__ = ("h",)

    def __init__(self, h):
        self.h = h


class Buf:
    __slots__ = ("w", "r", "psum")

    def __init__(self, psum=False):
        self.w = {}
        self.r = {}
        self.psum = psum


def bufs(n):
    return [Buf() for _ in range(n)]


def flat(x):
    if isinstance(x, Buf):
        return [x]
    out = []
    for y in x:
        if isinstance(y, Buf):
            out.append(y)
        else:
            out.extend(flat(y))
    return out


class Eng:
    def __init__(self, k, eng, is_pe=False):
        self.k = k
        self.eng = eng
        self.is_pe = is_pe
        self.sem = k.new_sem()
        self.cnt = 0
        self.waited = {}


class DQ:
    def __init__(self, k, eng, nsem, waited=None):
        self.k = k
        self.eng = eng
        self.sems = [[k.new_sem(), 0] for _ in range(nsem)]
        self.i = 0
        self.waited = {} if waited is None else waited


class Rot:
    def __init__(self, items):
        self.items = items
        self.i = 0

    def next(self):
        it = self.items[self.i % len(self.items)]
        self.i += 1
        return it


class K:
    def __init__(self, nc, es):
        self.nc = nc
        self.es = es
        self.nsem = 0
        self.PE = Eng(self, nc.tensor, is_pe=True)
        self.ACT = Eng(self, nc.scalar)
        self.DVE = Eng(self, nc.vector)
        self.POOL = Eng(self, nc.gpsimd)
        self.SY = DQ(self, nc.sync, 24)
        self.GQ = DQ(self, nc.gpsimd, 24, waited=self.POOL.waited)
        self.evi = 0

    def new_sem(self):
        self.nsem += 1
        return Sem(self.es.enter_context(self.nc.semaphore("s%d" % self.nsem)))

    def sb(self, name, shape, dt):
        return self.es.enter_context(self.nc.sbuf_tensor(un(name), shape, dt))

    def ps(self, name, shape, dt):
        return self.es.enter_context(self.nc.psum_tensor(un(name), shape, dt))

    @staticmethod
    def _deps(reads, writes):
        d = {}
        for b in reads:
            for s, v in b.w.items():
                if d.get(s, 0) < v:
                    d[s] = v
            if b.psum:
                for s, v in b.r.items():
                    if d.get(s, 0) < v:
                        d[s] = v
        for b in writes:
            for s, v in b.w.items():
                if d.get(s, 0) < v:
                    d[s] = v
            for s, v in b.r.items():
                if d.get(s, 0) < v:
                    d[s] = v
        return d

    @staticmethod
    def _wait(E, d):
        for s, v in d.items():
            if E.waited.get(s, 0) >= v:
                continue
            E.eng.wait_ge(s.h, v)
            E.waited[s] = v

    @staticmethod
    def _record(reads, writes, s, v):
        for b in reads:
            if b.r.get(s, 0) < v:
                b.r[s] = v
        for b in writes:
            b.w = {s: v}
            b.r = {}

    def op(self, E, fn, reads=(), writes=()):
        reads = flat(reads)
        writes = flat(writes)
        d = self._deps(reads, writes)
        if E.is_pe:
            d.pop(E.sem, None)
        self._wait(E, d)
        ins = fn(E.eng)
        if E.cnt >= 60000:
            E.sem = self.new_sem()
            E.cnt = 0
        E.cnt += 1
        ins.then_inc(E.sem.h, 1)
        self._record(reads, writes, E.sem, E.cnt)

    def dma(self, Q, out, in_, reads=(), writes=()):
        reads = flat(reads)
        writes = flat(writes)
        slot = Q.sems[Q.i % len(Q.sems)]
        Q.i += 1
        d = self._deps(reads, writes)
        if slot[1] > 0 and d.get(slot[0], 0) < slot[1]:
            d[slot[0]] = slot[1]
        self._wait(Q, d)
        Q.eng.dma_start(out=out, in_=in_).then_inc(slot[0].h, 16)
        slot[1] += 16
        self._record(reads, writes, slot[0], slot[1])

    def barrier(self):
        toks = {}
        for E in (self.PE, self.ACT, self.DVE):
            if E.cnt > 0:
                toks[E.sem] = E.cnt
        for Q in (self.SY, self.GQ):
            for s, c in Q.sems:
                if c > 0:
                    toks[s] = c
        for E in (self.PE, self.ACT, self.DVE, self.SY, self.GQ):
            self._wait(E, dict(toks))

    def evac_eng(self):
        self.evi += 1
        return self.ACT if (self.evi & 1) else self.DVE


class WStream:
    def __init__(self, slots, loader, total):
        self.slots = slots
        self.loader = loader
        self.total = total
        self.nxt = 0

    def get(self, i):
        lim = min(i + len(self.slots) - 1, self.total - 1)
        while self.nxt <= lim:
            self.loader(self.nxt, self.slots[self.nxt % len(self.slots)])
            self.nxt += 1
        return self.slots[i % len(self.slots)]


def na_tiles(p):
    lo = min(max(2 * p - 4, 0), 24)
    hi = min(max(2 * p + 1 - 4, 0), 24) + 7
    return list(range(lo // 2, hi // 2 + 1))


NA_VAR_P = [0, 1, 2, 14, 15]


def na_var(p):
    if p < 2:
        return p
    if p > 13:
        return p - 11
    return 2


def na_index_tables():
    idx = np.full((5, 128, 5, 128), 465, np.int64)
    for vi, p in enumerate(NA_VAR_P):
        tiles = na_tiles(p)
        for si, t in enumerate(tiles):
            for kp in range(128):
                kr = 2 * t + kp // 64
                kc = kp % 64
                for q in range(128):
                    r = 2 * p + q // 64
                    cq = q % 64
                    rs = min(max(r - 4, 0), 24)
                    cs = min(max(cq - 8, 0), 48)
                    if rs <= kr < rs + 8 and cs <= kc < cs + 16:
                        ri = kr - r + 7
                        ci = min(max(kc - cq, -15), 15) + 15
                        idx[vi, kp, si, q] = ri * 31 + ci
    return idx


_NA_IDX = None


def build(n_layers=L, debug=(), stop=None, skip=()):
    nc = bass.Bass("TRN2", target_bir_lowering=False)
    es = contextlib.ExitStack()
    dbg = set(debug)

    def din(name, shape, dt=F32):
        if name in skip:
            return None
        return nc.dram_tensor(name, list(shape), dt, kind="ExternalInput").ap()

    def dscr(name, shape, dt):
        kind = "ExternalOutput" if name in dbg else "Internal"
        return nc.dram_tensor(name, list(shape), dt, kind=kind).ap()

    x_in = din("x", [SEQ, D])
    ctx_in = din("ctx", [CTX, D])
    cvec_in = din("cvec", [128, 16, 2])
    wada_in = din("wada", [L, 24, 128, 16 * 512])
    bada_in = din("bada", [128, L, 96])
    g1_in = din("g1", [128, L, 16])
    g2_in = din("g2", [128, L, 16])
    win_in = din("win", [L, 16, 128, 16 * 512])
    wcg_in = din("wcg", [L, 128, 16 * 16])
    wgate_in = din("wgate", [L, 12, 128, 16 * 512])
    gsgu_in = din("gsgu", [L, 1024])
    wsp_in = din("wsp", [L, 128, 8 * 128])
    bsp_in = din("bsp", [L, 1024])
    natab_in = din("natab", [L, 8, 128, 5 * 5 * 128])
    rope_in = din("rope", [128, 4 * SEQ])
    consts_in = din("consts", [128, 5 * 128])
    bmg_in = din("bmg", [L, 16])
    gmn_in = din("gmn", [L, 1024])
    wbr_in = din("wbr", [L, 16, 128, 3 * 8 * 128])
    wout_in = din("wout", [L, 4, 128, 16 * 512])
    wr_in = din("wr", [128, 16 * 16])
    br_in = din("br", [16])
    weg_in = din("weg", [L, NE, 8, 128, 16 * 128])
    weu_in = din("weu", [L, NE, 8, 128, 16 * 128])
    wed_in = din("wed", [L, NE, 4, 128, 8 * 512])
    gfin_in = din("gfin", [D])
    y_out = nc.dram_tensor("y", [SEQ, D], F32, kind="ExternalOutput").ap()

    XL = dscr("XL", [NT, D], F32)
    AU_T = dscr("AU_T", [1024, NT], BF16)
    AV = dscr("AV", [NT, 1024], BF16)
    BQ_T = dscr("BQ_T", [1024, NT], BF16)
    BK_T = dscr("BK_T", [1024, NT], BF16)
    BV = dscr("BV", [NT, 1024], BF16)
    CQ_T = dscr("CQ_T", [512, NT], BF16)
    CK_T = dscr("CK_T", [512, NT], BF16)
    CV = dscr("CV", [NT, 1024], BF16)
    CO = dscr("CO", [NT, 1024], BF16)
    CG = dscr("CG", [NT, 16], F32)
    GATE_T = dscr("GATE_T", [6144, NT], BF16)
    YA_T = dscr("YA_T", [1024, NT], BF16)
    YB_T = dscr("YB_T", [1024, NT], BF16)
    YC_T = dscr("YC_T", [1024, NT], BF16)
    MG_T = dscr("MG_T", [D, NT], BF16)
    MODROW = dscr("MODROW", [L, 2, 96 * 128], F32)
    HD = dscr("HD", [D, NT], BF16)
    CMB = dscr("CMB", [NT, 16], F32)
    H2T = dscr("H2T", [D, NT], BF16)

    with es:
        k = K(nc, es)
        PE, ACT, DVE, SY, GQ = k.PE, k.ACT, k.DVE, k.SY, k.GQ

        mm = [(k.ps("mm%d" % i, [128, 512], F32), Buf(psum=True)) for i in range(4)]
        aux = [(k.ps("aux%d" % i, [128, 512], F32), Buf(psum=True)) for i in range(2)]
        tb = [(k.ps("tb%d" % i, [128, 1024], BF16), Buf(psum=True)) for i in range(2)]
        mmr, auxr, tbr = Rot(mm), Rot(aux), Rot(tb)

        BIGB = [bufs(16) for _ in range(NTILE)]

        class Scope:
            def __init__(self):
                self.es = contextlib.ExitStack()

            def __enter__(self):
                self.es.__enter__()
                return self

            def __exit__(self, *a):
                return self.es.__exit__(*a)

            def sb(self, name, shape, dt):
                return self.es.enter_context(nc.sbuf_tensor(un(name), shape, dt))

        def alloc_big(S):
            k.BIG = S.sb("BIG", [128, 16, NT], BF16)

        def alloc_wslot(S):
            k.wslot = [(S.sb("wslot%d" % i, [128, 16, 512], BF16), Buf()) for i in range(2)]

        def alloc_x(S):
            k.xt_r = Rot([(S.sb("xt%d" % i, [128, D], F32), Buf()) for i in range(2)])
            k.xn_r = Rot([(S.sb("xn%d" % i, [128, D], F32), Buf()) for i in range(2)])
        cst = k.sb("cst", [128, 5, 128], F32)
        cstb = Buf()
        identF = cst[:, 0, :]
        triF = [cst[:, 1, :], cst[:, 2, :]]
        onesF = cst[:, 3, :]
        identB = k.sb("identB", [128, 128], BF16)
        onesB = k.sb("onesB", [128, 128], BF16)
        permB = k.sb("permB", [128, 128], BF16)
        cbb = Buf()
        modF = k.sb("modF", [128, L, 96, 2], F32)
        modb = Buf()
        bada = k.sb("bada", [128, L, 96], F32)
        g1s = k.sb("g1s", [128, L, 16], F32)
        g2s = k.sb("g2s", [128, L, 16], F32)
        smallb = Buf()
        Gp = k.sb("Gp", [128, 16, 2], F32)
        Shp = k.sb("Shp", [128, 16, 2], F32)
        gpb = Buf()
        sc_r = Rot([(k.sb("sc%d" % i, [128, 8], F32), Buf()) for i in range(4)])

        k.dma(SY, cst[:].rearrange("p a b -> p (a b)"), consts_in[:, :], writes=[cstb])
        k.op(ACT, lambda e: e.activation(out=identB[:], in_=cst[:, 0, :], func=AF.Copy), reads=[cstb], writes=[cbb])
        k.op(ACT, lambda e: e.activation(out=onesB[:], in_=cst[:, 3, :], func=AF.Copy), reads=[cstb], writes=[cbb])
        k.op(ACT, lambda e: e.activation(out=permB[:], in_=cst[:, 4, :], func=AF.Copy), reads=[cstb], writes=[cbb])
        k.dma(SY, bada[:].rearrange("p a b -> p (a b)"), bada_in.rearrange("p a b -> p (a b)"), writes=[smallb])
        k.dma(SY, g1s[:].rearrange("p a b -> p (a b)"), g1_in.rearrange("p a b -> p (a b)"), writes=[smallb])
        k.dma(SY, g2s[:].rearrange("p a b -> p (a b)"), g2_in.rearrange("p a b -> p (a b)"), writes=[smallb])

        def evac(out, in_, reads, writes, E=None):
            E = E or k.evac_eng()
            if E is ACT:
                k.op(ACT, lambda e: e.activation(out=out, in_=in_, func=AF.Copy), reads=reads, writes=writes)
            else:
                k.op(DVE, lambda e: e.tensor_copy(out=out, in_=in_), reads=reads, writes=writes)

        cv = k.sb("cv", [128, 16, 2], F32)
        cvs = k.sb("cvs", [128, 16, 2], BF16)
        cvb = Buf()
        k.dma(SY, cv[:].rearrange("p a b -> p (a b)"), cvec_in.rearrange("p a b -> p (a b)"), writes=[cvb])
        k.op(ACT, lambda e: e.activation(out=cvs[:], in_=cv[:], func=AF.Silu), reads=[cvb], writes=[cvb])
        S0 = Scope()
        S0.__enter__()
        alloc_wslot(S0)
        alloc_x(S0)
        for l in range(n_layers):
            def ld_ada(i, slot, l=l):
                k.dma(GQ, slot[0][:].rearrange("p a b -> p (a b)"), wada_in[l, i], writes=[slot[1]])
            st = WStream(k.wslot, ld_ada, 24)
            pst, psb = auxr.next()
            for jb in range(24):
                w, wb_ = st.get(jb)
                for jj in range(4):
                    j = jb * 4 + jj
                    for kc in range(16):
                        k.op(PE, lambda e: e.matmul(pst[:, 2 * j:2 * j + 2], w[:, kc, jj * 128:(jj + 1) * 128], cvs[:, kc, :],
                                                    start=(kc == 0), stop=(kc == 15)),
                             reads=[wb_, cvb], writes=[psb])
            for v in range(2):
                k.op(DVE, lambda e: e.tensor_tensor(out=modF[:, l, :, v], in0=pst[:, 0:192].rearrange("p (j v) -> p j v", v=2)[:, :, v], in1=bada[:, l, :], op=ALU.add),
                     reads=[psb, smallb], writes=[modb])
            for v in range(2):
                pt, ptb = auxr.next()
                k.op(PE, lambda e: e.transpose(pt[0:96, 0:128], modF[:, l, :, v], identF), reads=[modb, cstb], writes=[ptb])
                mr, mrb = k.xn_r.next()
                evac(mr[0:96, 0:128], pt[0:96, 0:128], [ptb], [mrb])
                k.dma(SY, MODROW[l, v].rearrange("(j p) -> j p", p=128), mr[0:96, 0:128], reads=[mrb], writes=[modb])
        k.barrier()
        S0.__exit__(None, None, None)

        def x_src(l, which, m):
            if l == 0 and which == 1:
                return (x_in[m * 128:(m + 1) * 128, :] if m < 16 else ctx_in[(m - 16) * 128:(m - 15) * 128, :]), []
            return XL[m * 128:(m + 1) * 128, :], XB[m]

        XB = [bufs(4) for _ in range(NTILE)]
        comb = k.sb("comb", [128, NTILE, 16], F32)
        combb = bufs(NTILE)
        wr_s = k.sb("wr_s", [128, 16, 16], F32)
        br_s = k.sb("br_s", [128, 16], F32)
        k.dma(SY, wr_s[:].rearrange("p a b -> p (a b)"), wr_in[:, :], writes=[smallb])
        k.dma(SY, br_s[:], br_in.partition_broadcast(128), writes=[smallb])

        epsT = k.sb("epsT", [128, 2], F32)
        epsb = Buf()
        k.op(DVE, lambda e: e.memset(epsT[:, 0:1], EPS), writes=[epsb])
        k.op(DVE, lambda e: e.memset(epsT[:, 1:2], 1.0), writes=[epsb])

        def rstd_of(ssq, ssb, n):
            k.op(ACT, lambda e: e.activation(out=ssq, in_=ssq, func=AF.Ln, scale=1.0 / n, bias=epsT[:, 0:1]), reads=[ssb, epsb], writes=[ssb])
            k.op(ACT, lambda e: e.activation(out=ssq, in_=ssq, func=AF.Exp, scale=-0.5), reads=[ssb], writes=[ssb])

        def phase_norm(l, which, tiles, router=False):
            gsrc = g1s if which == 1 else g2s
            jsh, jsc = (0, 16) if which == 1 else (48, 64)
            for v in range(2):
                k.op(DVE, lambda e: e.scalar_tensor_tensor(out=Gp[:, :, v], in0=modF[:, l, jsc:jsc + 16, v], scalar=1.0,
                                                           in1=gsrc[:, l, :], op0=ALU.add, op1=ALU.mult),
                     reads=[modb, smallb], writes=[gpb])
                k.op(DVE, lambda e: e.tensor_copy(out=Shp[:, :, v], in_=modF[:, l, jsh:jsh + 16, v]), reads=[modb], writes=[gpb])
            pend_ld = {}

            def issue_ld(m_):
                xt_, xtb_ = k.xt_r.next()
                src_, srcb_ = x_src(l, which, m_)
                k.dma(SY, xt_[:], src_, reads=srcb_, writes=[xtb_])
                pend_ld[m_] = (xt_, xtb_)
            issue_ld(tiles[0])
            deferred = []
            for idx_, m in enumerate(tiles):
                v = 0 if m < 16 else 1
                if idx_ + 1 < len(tiles):
                    issue_ld(tiles[idx_ + 1])
                xt, xtb = pend_ld.pop(m)
                xn, xnb = k.xn_r.next()
                ss, ssb = sc_r.next()
                k.op(DVE, lambda e: e.memset(ss[:, 0:1], 0.0), writes=[ssb])
                k.op(ACT, lambda e: e.activation(out=xn[:], in_=xt[:], func=AF.Square, accum_out=ss[:, 0:1]),
                     reads=[xtb], writes=[xnb, ssb])
                rstd_of(ss[:, 0:1], ssb, D)
                if router:
                    k.op(ACT, lambda e: e.activation(out=xn[:], in_=xt[:], func=AF.Copy, scale=ss[:, 0:1]),
                         reads=[xtb, ssb], writes=[xnb])
                else:
                    k.op(DVE, lambda e: e.tensor_scalar(out=xn[:], in0=xt[:], scalar1=ss[:, 0:1], scalar2=None, op0=ALU.mult),
                         reads=[xtb, ssb], writes=[xnb])
                if router:
                    hf = k.hf_r.next()
                    while deferred:
                        deferred.pop(0)()
                for q4 in range(4):
                    pt, ptb = auxr.next()
                    for i in range(4):
                        kc = q4 * 4 + i
                        k.op(PE, lambda e: e.transpose(pt[:, i * 128:(i + 1) * 128], xn[:, kc * 128:(kc + 1) * 128], identF),
                             reads=[xnb, cstb], writes=[ptb])
                    for i in range(4):
                        kc = q4 * 4 + i
                        if router:
                            if i == 0 and q4 == 0:
                                h2s, h2b = k.h2s_r.next()
                            dst = h2s[:, kc, :]
                            dstb = h2b[kc]
                        else:
                            dst = k.BIG[:, kc, m * 128:(m + 1) * 128]
                            dstb = BIGB[m][kc]
                        if router:
                            k.op(DVE, lambda e: e.tensor_scalar(out=hf[0][:, kc, :], in0=pt[:, i * 128:(i + 1) * 128],
                                                                scalar1=Gp[:, kc, v:v + 1], scalar2=Shp[:, kc, v:v + 1],
                                                                op0=ALU.mult, op1=ALU.add),
                                 reads=[ptb, gpb], writes=[hf[1][kc]])
                            k.op(ACT, lambda e: e.activation(out=dst, in_=hf[0][:, kc, :], func=AF.Copy),
                                 reads=[hf[1][kc]], writes=[dstb])
                        else:
                            E = ACT if (q4 & 1) == 0 else DVE
                            if E is ACT:
                                k.op(ACT, lambda e: e.activation(out=dst, in_=pt[:, i * 128:(i + 1) * 128], func=AF.Identity,
                                                                 scale=Gp[:, kc, v:v + 1], bias=Shp[:, kc, v:v + 1]),
                                     reads=[ptb, gpb], writes=[dstb])
                            else:
                                k.op(DVE, lambda e: e.tensor_scalar(out=dst, in0=pt[:, i * 128:(i + 1) * 128],
                                                                    scalar1=Gp[:, kc, v:v + 1], scalar2=Shp[:, kc, v:v + 1],
                                                                    op0=ALU.mult, op1=ALU.add),
                                     reads=[ptb, gpb], writes=[dstb])
                if router:
                    def back(m=m, hf=hf, h2s=h2s, h2b=h2b):
                        pr, prb = mmr.next()
                        for kc in range(16):
                            k.op(PE, lambda e: e.matmul(pr[:, 0:16], hf[0][:, kc, :], wr_s[:, kc, :], start=(kc == 0), stop=(kc == 15)),
                                 reads=[hf[1][kc], smallb], writes=[prb])
                        route(m, pr, prb)
                        k.dma(SY, H2T.rearrange("(c p) t -> p c t", p=128)[:, :, m * 128:(m + 1) * 128], h2s[:], reads=[h2b], writes=[])
                    deferred.append(back)
            while deferred:
                deferred.pop(0)()

        def route(m, pr, prb):
            R, Rb = k.rt_r.next()
            k.op(ACT, lambda e: e.activation(out=R[:, 0:16], in_=pr[:, 0:16], func=AF.Exp, scale=-1.0), reads=[prb], writes=[Rb])
            k.op(DVE, lambda e: e.tensor_scalar(out=R[:, 0:16], in0=R[:, 0:16], scalar1=1.0, scalar2=None, op0=ALU.add), reads=[Rb], writes=[Rb])
            k.op(DVE, lambda e: e.reciprocal(out=R[:, 0:16], in_=R[:, 0:16]), reads=[Rb], writes=[Rb])
            k.op(DVE, lambda e: e.tensor_tensor(out=R[:, 16:32], in0=R[:, 0:16], in1=br_s[:], op=ALU.add), reads=[Rb, smallb], writes=[Rb])
            sel = R[:, 16:32].rearrange("p (g e) -> p g e", e=4)
            prs = R[:, 32:56].rearrange("p (g e) -> p g e", e=6)
            pairs = [(0, 1), (0, 2), (0, 3), (1, 2), (1, 3), (2, 3)]
            for pi, (a, b) in enumerate(pairs):
                k.op(DVE, lambda e: e.tensor_tensor(out=prs[:, :, pi], in0=sel[:, :, a], in1=sel[:, :, b], op=ALU.add), reads=[Rb], writes=[Rb])
            k.op(DVE, lambda e: e.tensor_reduce(out=R[:, 96:97], in_=R[:, 32:56], axis=mybir.AxisListType.X, op=ALU.max), reads=[Rb], writes=[Rb])
            k.op(DVE, lambda e: e.tensor_scalar(out=R[:, 56:80], in0=R[:, 32:56], scalar1=R[:, 96:97], scalar2=None, op0=ALU.is_equal),
                 reads=[Rb], writes=[Rb])
            oh = R[:, 56:80].rearrange("p (g e) -> p g e", e=6)
            sl = R[:, 80:96].rearrange("p (g e) -> p g e", e=4)
            members = {0: (0, 1, 2), 1: (0, 3, 4), 2: (1, 3, 5), 3: (2, 4, 5)}
            for ei, (a, b, c) in members.items():
                k.op(DVE, lambda e: e.tensor_tensor(out=sl[:, :, ei], in0=oh[:, :, a], in1=oh[:, :, b], op=ALU.add), reads=[Rb], writes=[Rb])
                k.op(DVE, lambda e: e.tensor_tensor(out=sl[:, :, ei], in0=sl[:, :, ei], in1=oh[:, :, c], op=ALU.add), reads=[Rb], writes=[Rb])
            k.op(DVE, lambda e: e.tensor_tensor(out=R[:, 80:96], in0=R[:, 80:96], in1=R[:, 0:16], op=ALU.mult), reads=[Rb], writes=[Rb])
            k.op(DVE, lambda e: e.tensor_reduce(out=R[:, 97:98], in_=R[:, 80:96], axis=mybir.AxisListType.X, op=ALU.add), reads=[Rb], writes=[Rb])
            k.op(DVE, lambda e: e.reciprocal(out=R[:, 97:98], in_=R[:, 97:98]), reads=[Rb], writes=[Rb])
            k.op(DVE, lambda e: e.tensor_scalar(out=comb[:, m, :], in0=R[:, 80:96], scalar1=R[:, 97:98], scalar2=None, op0=ALU.mult),
                 reads=[Rb], writes=[combb[m]])
            if "CMB" in dbg:
                k.dma(SY, CMB[m * 128:(m + 1) * 128, :], comb[:, m, :], reads=[combb[m]], writes=[])

        def alloc_st(S):
            k.stF_r = Rot([(S.sb("stF%d" % i, [128, NT], BF16), bufs(5)) for i in range(2)])
            k.stT_r = Rot([(S.sb("stT%d" % i, [128, 512], BF16), Buf()) for i in range(3)])
        TG = [(0, 512), (512, 512), (1024, 512), (1536, 512), (2048, 256)]

        def proj_F(w, wb_, dst, r0, ncol=4, tgs=TG, scale=None):
            for jj in range(ncol):
                stF, stb = k.stF_r.next()
                for tgi_, (t0, tn) in enumerate(tgs):
                    pt, ptb = mmr.next()
                    for kc in range(16):
                        k.op(PE, lambda e: e.matmul(pt[:, 0:tn], w[:, kc, jj * 128:(jj + 1) * 128], k.BIG[:, kc, t0:t0 + tn],
                                                    start=(kc == 0), stop=(kc == 15)),
                             reads=[wb_, [BIGB[mm_][kc] for mm_ in range(t0 // 128, (t0 + tn) // 128)]], writes=[ptb])
                    if scale is None:
                        evac(stF[:, t0:t0 + tn], pt[:, 0:tn], [ptb], [stb[tgi_]])
                    else:
                        k.op(ACT, lambda e: e.activation(out=stF[:, t0:t0 + tn], in_=pt[:, 0:tn], func=AF.Copy, scale=scale), reads=[ptb], writes=[stb[tgi_]])
                tend = tgs[-1][0] + tgs[-1][1]
                k.dma(SY, dst[r0 + jj * 128:r0 + (jj + 1) * 128, 0:tend], stF[:, 0:tend], reads=[stb], writes=[])
                yield

        def proj_T(w, wb_, dst, c0, tiles):
            for m in tiles:
                pt, ptb = mmr.next()
                for kc in range(16):
                    k.op(PE, lambda e: e.matmul(pt[:, :], k.BIG[:, kc, m * 128:(m + 1) * 128], w[:, kc, :], start=(kc == 0), stop=(kc == 15)),
                         reads=[wb_, BIGB[m][kc]], writes=[ptb])
                stT, stb = k.stT_r.next()
                evac(stT[:, :], pt[:, :], [ptb], [stb])
                k.dma(SY, dst[m * 128:(m + 1) * 128, c0:c0 + 512], stT[:, :], reads=[stb], writes=[])
                if m % 4 == 3:
                    yield

        MAIN = [("F", AU_T, 0), ("F", AU_T, 512), ("T", AV, 0), ("T", AV, 512), ("F", BQ_T, 0), ("F", BQ_T, 512),
                ("F", BK_T, 0), ("F", BK_T, 512), ("T", BV, 0), ("T", BV, 512), ("F", CQ_T, 0), ("F", CK_T, 0),
                ("T", CV, 0), ("T", CV, 512), ("T", CO, 0), ("T", CO, 512)]

        def phase_inproj(l, S, last=False):
            tiles = list(range(NTILE))
            lat_only = {id(AU_T), id(BQ_T), id(CQ_T), id(AV), id(CO)} if last else set()
            bmg = S.sb("bmg", [128, 16], F32)
            wcg = S.sb("wcg", [128, 16, 16], BF16)
            cgs = S.sb("cgs", [128, NTILE, 16], F32)
            tmpb = Buf()
            k.dma(SY, bmg[:], bmg_in[l].partition_broadcast(128), writes=[tmpb])
            k.dma(GQ, wcg[:].rearrange("p a b -> p (a b)"), wcg_in[l], writes=[tmpb])

            def ld(i, slot):
                src = win_in[l, i] if i < 16 else wgate_in[l, i - 16]
                k.dma(GQ, slot[0][:].rearrange("p a b -> p (a b)"), src, writes=[slot[1]])
            st = WStream(k.wslot, ld, 28)
            for i in range(28):
                if i == 4:
                    yield "AUAV"
                w, wb_ = st.get(i)
                if i < 16:
                    mode, dst, off = MAIN[i]
                    lo_ = id(dst) in lat_only
                    if mode == "F":
                        yield from proj_F(w, wb_, dst, off, tgs=(TG[:4] if lo_ else TG), scale=(NA_SCALE if dst is BQ_T else None))
                    else:
                        yield from proj_T(w, wb_, dst, off, (list(range(16)) if lo_ else tiles))
                else:
                    yield from proj_F(w, wb_, GATE_T, (i - 16) * 512, tgs=(TG[:4] if last else TG))
            for m in tiles:
                pt, ptb = auxr.next()
                for kc in range(16):
                    k.op(PE, lambda e: e.matmul(pt[:, 0:16], k.BIG[:, kc, m * 128:(m + 1) * 128], wcg[:, kc, :], start=(kc == 0), stop=(kc == 15)),
                         reads=[tmpb, BIGB[m][kc]], writes=[ptb])
                k.op(DVE, lambda e: e.tensor_tensor(out=cgs[:, m, :], in0=pt[:, 0:16], in1=bmg[:], op=ALU.add), reads=[ptb, tmpb], writes=[tmpb])
            k.dma(SY, CG.rearrange("(m p) g -> p m g", p=128), cgs[:], reads=[tmpb], writes=[])

        def fence_bufs():
            b = Buf()
            for s, c in SY.sems:
                if c > 0:
                    b.w[s] = c
            for s, c in GQ.sems:
                if c > 0:
                    b.w[s] = c
            return b

        def gelu_(dst, src, tmp, reads, wr):
            k.op(ACT, lambda e: e.activation(out=tmp, in_=src, func=AF.Square), reads=reads, writes=[wr])
            k.op(DVE, lambda e: e.scalar_tensor_tensor(out=tmp, in0=tmp, scalar=1.0 / 0.044715, in1=src, op0=ALU.add, op1=ALU.mult), reads=[wr] + reads, writes=[wr])
            k.op(ACT, lambda e: e.activation(out=tmp, in_=tmp, func=AF.Sigmoid, scale=GELU_C * 0.044715), reads=[wr], writes=[wr])
            k.op(DVE, lambda e: e.tensor_tensor(out=dst, in0=tmp, in1=src, op=ALU.mult), reads=[wr] + reads, writes=[wr])

        def gelu2(items):
            for (dst, src, tmp, reads, wr) in items:
                k.op(ACT, lambda e: e.activation(out=tmp, in_=src, func=AF.Square), reads=reads, writes=[wr])
            for (dst, src, tmp, reads, wr) in items:
                k.op(DVE, lambda e: e.scalar_tensor_tensor(out=tmp, in0=tmp, scalar=1.0 / 0.044715, in1=src, op0=ALU.add, op1=ALU.mult), reads=[wr] + reads, writes=[wr])
            for (dst, src, tmp, reads, wr) in items:
                k.op(ACT, lambda e: e.activation(out=tmp, in_=tmp, func=AF.Sigmoid, scale=GELU_C * 0.044715), reads=[wr], writes=[wr])
            for (dst, src, tmp, reads, wr) in items:
                k.op(DVE, lambda e: e.tensor_tensor(out=dst, in0=tmp, in1=src, op=ALU.mult), reads=[wr] + reads, writes=[wr])

        def phase_sgu(l, fb, tiles, sb, Q=None):
            Q = Q or SY
            if True:
                gs = sb("gs", [128, 1024], F32)
                wspf = sb("wspf", [128, 8, 128], F32)
                wspb = sb("wspb", [128, 8, 128], BF16)
                bspf = sb("bspf", [1, 1024], F32)
                bspb = sb("bspb", [1, 1024], BF16)
                cb = Buf()
                k.dma(Q, gs[:], gsgu_in[l].partition_broadcast(128), writes=[cb])
                k.dma(Q, wspf[:].rearrange("p a b -> p (a b)"), wsp_in[l], writes=[cb])
                k.dma(Q, bspf[:], bsp_in[l:l + 1, :], writes=[cb])
                k.op(ACT, lambda e: e.activation(out=wspb[:], in_=wspf[:], func=AF.Copy), reads=[cb], writes=[cb])
                k.op(ACT, lambda e: e.activation(out=bspb[:], in_=bspf[:], func=AF.Copy), reads=[cb], writes=[cb])
                avr = Rot([(sb("av%d" % i, [128, 1024], BF16), Buf()) for i in range(2)])
                aur = Rot([(sb("au%d" % i, [128, 8, 128], BF16), Buf()) for i in range(2)])
                t1r = Rot([(sb("t1%d" % i, [128, 1024], F32), Buf()) for i in range(2)])
                gvr = Rot([(sb("gv%d" % i, [128, 1024], F32), Buf()) for i in range(2)])
                gur = Rot([(sb("gu%d" % i, [128, 1024], F32), Buf()) for i in range(2)])
                vnr = Rot([(sb("vn%d" % i, [128, 1024], BF16), Buf()) for i in range(2)])
                yar = Rot([(sb("ya%d" % i, [128, 8, 128], BF16), Buf()) for i in range(2)])
                AUv = AU_T.rearrange("(g p) t -> p g t", p=128)
                YAv = YA_T.rearrange("(g p) t -> p g t", p=128)
                sld = {}
                spend = []

                def sgu_mm(c, vn, vnb, gu, t2b):
                    ya, yab = yar.next()
                    for half in range(2):
                        pt, ptb = auxr.next()
                        for gi in range(4):
                            g = half * 4 + gi
                            k.op(PE, lambda e: e.matmul(pt[:, gi * 128:(gi + 1) * 128], vn[:, g * 128:(g + 1) * 128], wspb[:, g, :], start=True, stop=False),
                                 reads=[vnb, cb], writes=[ptb])
                            k.op(PE, lambda e: e.matmul(pt[:, gi * 128:(gi + 1) * 128], onesB[0:1, :], bspb[0:1, g * 128:(g + 1) * 128], start=False, stop=True),
                                 reads=[cbb, cb], writes=[ptb])
                        k.op(DVE, lambda e: e.tensor_tensor(out=ya[:, half * 4:half * 4 + 4, :].rearrange("p a b -> p (a b)"), in0=pt[:, :],
                                                            in1=gu[:, half * 512:(half + 1) * 512], op=ALU.mult),
                             reads=[ptb, t2b], writes=[yab])
                    k.dma(Q, YAv[:, :, c * 128:(c + 1) * 128], ya[:], reads=[yab], writes=[])

                def issue_s(c_):
                    av_, avb_ = avr.next()
                    au_, aub_ = aur.next()
                    k.dma(Q, av_[:], AV[c_ * 128:(c_ + 1) * 128, :], reads=[fb], writes=[avb_])
                    k.dma(Q, au_[:], AUv[:, :, c_ * 128:(c_ + 1) * 128], reads=[fb], writes=[aub_])
                    sld[c_] = (av_, avb_, au_, aub_)
                issue_s(tiles[0])
                for ci_, c in enumerate(tiles):
                    if ci_ + 1 < len(tiles):
                        issue_s(tiles[ci_ + 1])
                    av, avb, au, aub = sld.pop(c)
                    t1, t1b = t1r.next()
                    gv, gvb = gvr.next()
                    gu, gub = gur.next()
                    t2, t2b = t1r.next()
                    gelu2([(gv[:], av[:], t1[:], [avb], t1b), (gu[:], au[:].rearrange("p a b -> p (a b)"), t2[:], [aub], t2b)])
                    ss, ssb = sc_r.next()
                    k.op(DVE, lambda e: e.memset(ss[:, 0:1], 0.0), writes=[ssb])
                    k.op(ACT, lambda e: e.activation(out=t1[:], in_=gv[:], func=AF.Square, accum_out=ss[:, 0:1]), reads=[t1b], writes=[t1b, ssb])
                    rstd_of(ss[:, 0:1], ssb, 1024)
                    vn, vnb = vnr.next()
                    k.op(DVE, lambda e: e.scalar_tensor_tensor(out=vn[:], in0=gv[:], scalar=ss[:, 0:1], in1=gs[:], op0=ALU.mult, op1=ALU.mult),
                         reads=[t1b, ssb, cb], writes=[vnb])
                    if spend:
                        sgu_mm(*spend.pop())
                    spend.append((c, vn, vnb, gu, t2b))
                    yield
                sgu_mm(*spend.pop())
                yield

        def phase_na(l, fb, with_ctx_q, sb):
            hbufs = Rot([dict(q=sb("naq%d" % i, [128, NT], BF16), k=sb("nak%d" % i, [128, NT], BF16), v=sb("nav%d" % i, [128, NTILE, 128], BF16),
                              b=Buf()) for i in range(2)])
            Ht = sb("nat", [128, 25, 128], BF16)
            htb = Buf()
            yr_ = Rot([(sb("nay%d" % i, [128, NT], BF16), Buf()) for i in range(2)])
            tmr = Rot([(sb("natm%d" % i, [128, 5 * 128], F32), Buf()) for i in range(2)])
            ptr_ = Rot([(sb("napt%d" % i, [128, 7 * 128], BF16), Buf()) for i in range(3)])
            rdr = Rot([(sb("nard%d" % i, [128, 128], F32), Buf()) for i in range(2)])
            BVv = BV.rearrange("(m p) f -> p m f", p=128)
            nq = NTILE if with_ctx_q else 16
            def ld_head(h_):
                H_ = hbufs.next()
                k.dma(SY, H_["q"][:], BQ_T[h_ * 128:(h_ + 1) * 128, :], reads=[fb], writes=[H_["b"]])
                k.dma(SY, H_["k"][:], BK_T[h_ * 128:(h_ + 1) * 128, :], reads=[fb], writes=[H_["b"]])
                k.dma(SY, H_["v"][:], BVv[:, :, h_ * 128:(h_ + 1) * 128], reads=[fb], writes=[H_["b"]])
                return H_
            nxt_head = ld_head(0)
            for h in range(8):
                HH = nxt_head
                if h + 1 < 8:
                    nxt_head = ld_head(h + 1)
                Hq, Hk, Hv, hb = HH["q"], HH["k"], HH["v"], HH["b"]
                k.dma(GQ, Ht[:].rearrange("p a b -> p (a b)"), natab_in[l, h], writes=[htb])
                Hy, Hyb = yr_.next()

                def stageA(p):
                    if p < 16:
                        wt = na_tiles(p)
                        var = na_var(p)
                    else:
                        wt = []
                        var = 0
                    tiles = wt + [16, 17]
                    nw = len(wt)
                    pa, pab = mm[0]
                    pb_, pbb = mm[1]

                    def sreg(i):
                        return (pa, pab, i) if i < 4 else (pb_, pbb, i - 4)
                    for i, t in enumerate(tiles):
                        pt, ptb, ii = sreg(i)
                        isw = i < nw
                        k.op(PE, lambda e: e.matmul(pt[:, ii * 128:(ii + 1) * 128], Hk[:, t * 128:(t + 1) * 128], Hq[:, p * 128:(p + 1) * 128],
                                                    start=True, stop=(not isw)), reads=[hb], writes=[ptb])
                        if isw:
                            k.op(PE, lambda e: e.matmul(pt[:, ii * 128:(ii + 1) * 128], identB[:, :], Ht[:, var * 5 + i, :], start=False, stop=True),
                                 reads=[cbb, htb], writes=[ptb])
                    pT, pTb = ptr_.next()
                    nt_ = len(tiles)
                    na_ = min(nt_, 4)
                    k.op(ACT, lambda e: e.activation(out=pT[:, 0:na_ * 128], in_=pa[:, 0:na_ * 128], func=AF.Exp), reads=[pab], writes=[pTb])
                    if nt_ > 4:
                        k.op(ACT, lambda e: e.activation(out=pT[:, 512:nt_ * 128], in_=pb_[:, 0:(nt_ - 4) * 128], func=AF.Exp), reads=[pbb], writes=[pTb])
                    return (p, tiles, pT, pTb)

                def stageB(st):
                    p, tiles, pT, pTb = st
                    po, pob = aux[0]
                    nt_ = len(tiles)
                    for i, t in enumerate(tiles):
                        k.op(PE, lambda e: e.matmul(po[:, 0:128], Hv[:, t, :], pT[:, i * 128:(i + 1) * 128], start=(i == 0), stop=(i == nt_ - 1)),
                             reads=[hb, pTb], writes=[pob])
                    for i, t in enumerate(tiles):
                        k.op(PE, lambda e: e.matmul(po[:, 128:256], onesB[:, :], pT[:, i * 128:(i + 1) * 128], start=(i == 0), stop=(i == nt_ - 1)),
                             reads=[cbb, pTb], writes=[pob])
                    rd, rdb = rdr.next()
                    k.op(DVE, lambda e: e.reciprocal(out=rd[:], in_=po[:, 128:256]), reads=[pob], writes=[rdb])
                    k.op(DVE, lambda e: e.tensor_tensor(out=Hy[:, p * 128:(p + 1) * 128], in0=po[:, 0:128], in1=rd[:], op=ALU.mult),
                         reads=[pob, rdb], writes=[Hyb])
                pend = None
                for p in range(nq):
                    cur = stageA(p)
                    if pend is not None:
                        stageB(pend)
                        yield
                    pend = cur
                stageB(pend)
                k.dma(SY, YB_T[h * 128:(h + 1) * 128, 0:nq * 128], Hy[:, 0:nq * 128], reads=[Hyb], writes=[])
                yield

        def phase_rope(fb):
            with contextlib.ExitStack() as les:
                def sb(name, shape, dt):
                    return les.enter_context(nc.sbuf_tensor(un(name), shape, dt))
                rope = sb("rope", [128, 4, SEQ], F32)
                gb = Buf()
                k.dma(SY, rope[:].rearrange("p a b -> p (a b)"), rope_in[:, :], writes=[gb])
                rawr = Rot([(sb("rraw%d" % i, [128, NT], BF16), Buf()) for i in range(2)])
                dstr = Rot([(sb("rdst%d" % i, [128, NT], BF16), Buf()) for i in range(2)])
                rt1 = Rot([(sb("mrt%d" % i, [128, 512], F32), Buf()) for i in range(2)])
                rt2 = Rot([(sb("mru%d" % i, [128, 512], F32), Buf()) for i in range(2)])
                rjobs = [(h, T_, ti, isq) for h in range(4) for (T_, ti, isq) in ((CQ_T, 0, True), (CK_T, 2, False))]
                rpend = {}

                def issue_rope(ji):
                    h_, T__, _, _ = rjobs[ji]
                    raw_, rb_ = rawr.next()
                    k.dma(SY, raw_[:], T__[h_ * 128:(h_ + 1) * 128, :], reads=[fb], writes=[rb_])
                    rpend[ji] = (raw_, rb_)
                issue_rope(0)
                for ji, (h, T_, ti, isq) in enumerate(rjobs):
                    if True:
                        if ji + 1 < len(rjobs):
                            issue_rope(ji + 1)
                        raw, rb = rpend.pop(ji)
                        dst, db = dstr.next()
                        for tg in range(4):
                            sl = slice(tg * 512, (tg + 1) * 512)
                            pt, ptb = mmr.next()
                            k.op(PE, lambda e: e.matmul(pt[:, :], permB[:, :], raw[:, sl], start=True, stop=True), reads=[rb, cbb], writes=[ptb])
                            a, ab = rt1.next()
                            b, bb = rt2.next()
                            k.op(DVE, lambda e: e.tensor_tensor(out=a[:], in0=pt[:, :], in1=rope[:, ti + 1, sl], op=ALU.mult), reads=[ptb, gb], writes=[ab])
                            k.op(DVE, lambda e: e.tensor_tensor(out=b[:], in0=raw[:, sl], in1=rope[:, ti, sl], op=ALU.mult), reads=[rb, gb], writes=[bb])
                            k.op(DVE, lambda e: e.tensor_tensor(out=dst[:, sl], in0=a[:], in1=b[:], op=ALU.add), reads=[ab, bb], writes=[db])
                        if isq:
                            k.op(ACT, lambda e: e.activation(out=dst[:, SEQ:NT], in_=raw[:, SEQ:NT], func=AF.Copy, scale=NA_SCALE), reads=[rb], writes=[db])
                        else:
                            k.op(ACT, lambda e: e.activation(out=dst[:, SEQ:NT], in_=raw[:, SEQ:NT], func=AF.Copy), reads=[rb], writes=[db])
                        k.dma(SY, T_[h * 128:(h + 1) * 128, :], dst[:], reads=[db, rb], writes=[])

        def phase_mlstm(l, fb, ctx_out, sb):
            gmn = sb("mgmn", [128, 1024], F32)
            G = sb("mG", [128, NTILE, 16], F32)
            T1 = sb("mT1", [128, NTILE, 16], F32)
            LF = sb("mLF", [128, NTILE, 16], F32)
            CUM = sb("mCUM", [128, NTILE, 48], F32)
            A = sb("mA", [128, NTILE, 8], F32)
            KS = sb("mKS", [128, NTILE, 8], F32)
            DEC = sb("mDEC", [128, NTILE, 8], F32)
            AINV = sb("mAINV", [128, NTILE, 8], F32)
            KSD = sb("mKSD", [128, NTILE, 8], F32)
            gb = Buf()
            k.dma(SY, gmn[:], gmn_in[l].partition_broadcast(128), writes=[gb])
            k.dma(SY, G[:], CG.rearrange("(m p) g -> p m g", p=128), reads=[fb], writes=[gb])
            k.op(ACT, lambda e: e.activation(out=T1[:], in_=G[:], func=AF.Exp, scale=-1.0), reads=[gb], writes=[gb])
            k.op(DVE, lambda e: e.tensor_scalar(out=T1[:], in0=T1[:], scalar1=1.0, scalar2=None, op0=ALU.add), reads=[gb], writes=[gb])
            k.op(ACT, lambda e: e.activation(out=T1[:], in_=T1[:], func=AF.Ln), reads=[gb], writes=[gb])
            k.op(DVE, lambda e: e.tensor_scalar(out=LF[:], in0=T1[:], scalar1=-1.0, scalar2=None, op0=ALU.mult), reads=[gb], writes=[gb])
            for c in range(NTILE):
                pt, ptb = auxr.next()
                k.op(PE, lambda e: e.matmul(pt[:, 0:16], triF[0], LF[:, c, :], start=True, stop=True), reads=[cstb, gb], writes=[ptb])
                k.op(PE, lambda e: e.matmul(pt[:, 16:32], triF[1], LF[:, c, :], start=True, stop=True), reads=[cstb, gb], writes=[ptb])
                k.op(PE, lambda e: e.matmul(pt[:, 32:48], onesF, LF[:, c, :], start=True, stop=True), reads=[cstb, gb], writes=[ptb])
                k.op(DVE, lambda e: e.tensor_copy(out=CUM[:, c, :], in_=pt[:, 0:48]), reads=[ptb], writes=[gb])
            for d_, (bo, io, to) in enumerate([(4, 0, 36), (28, 8, 44)]):
                k.op(ACT, lambda e: e.activation(out=A[:, :, d_ * 4:d_ * 4 + 4], in_=CUM[:, :, bo:bo + 4], func=AF.Exp), reads=[gb], writes=[gb])
                k.op(ACT, lambda e: e.activation(out=AINV[:, :, d_ * 4:d_ * 4 + 4], in_=CUM[:, :, bo:bo + 4], func=AF.Exp, scale=-1.0), reads=[gb], writes=[gb])
                k.op(DVE, lambda e: e.tensor_tensor(out=KS[:, :, d_ * 4:d_ * 4 + 4], in0=G[:, :, io:io + 4], in1=CUM[:, :, bo:bo + 4], op=ALU.subtract),
                     reads=[gb], writes=[gb])
                k.op(ACT, lambda e: e.activation(out=KS[:, :, d_ * 4:d_ * 4 + 4], in_=KS[:, :, d_ * 4:d_ * 4 + 4], func=AF.Exp), reads=[gb], writes=[gb])
                k.op(ACT, lambda e: e.activation(out=DEC[:, :, d_ * 4:d_ * 4 + 4], in_=CUM[:, :, to:to + 4], func=AF.Exp), reads=[gb], writes=[gb])
            k.op(DVE, lambda e: e.tensor_tensor(out=KSD[:], in0=KS[:], in1=DEC[:], op=ALU.mult), reads=[gb], writes=[gb])
            yield
            Hs = sb("mH", [128, NTILE, 256], F32)
            sg32 = sb("msg32", [128, NTILE, 256], F32)
            sgb32 = Buf()
            ss18 = sb("mss18", [128, NTILE], F32)
            ss18b = Buf()
            junk = sb("mjunk", [128, 256], F32)
            junkb = Buf()
            Hb = bufs(NTILE)
            mbufs = Rot([dict(q=sb("mqT%d" % i, [128, NT], BF16), k=sb("mkT%d" % i, [128, NT], BF16), v=sb("mVp%d" % i, [128, NTILE, 257], BF16),
                              c=sb("mco%d" % i, [128, NTILE, 256], BF16), b=Buf()) for i in range(2)])
            spr = Rot([(sb("msp%d" % i, [128, 128], BF16), Buf()) for i in range(4)])
            ktr = Rot([(sb("mkt%d" % i, [128, 128], BF16), Buf()) for i in range(4)])
            tcr = Rot([(sb("mtc%d" % i, [128, 257], F32), Buf()) for i in range(2)])
            Cst = [(sb("mCs%d" % i, [128, 257], F32), Buf()) for i in range(2)]
            Cbf = [(sb("mCb%d" % i, [128, 257], BF16), Buf()) for i in range(2)]
            yr = Rot([(sb("my%d" % i, [128, 256], F32), Buf()) for i in range(2)])
            sgr_ = Rot([(sb("mys%d" % i, [128, 256], F32), Buf()) for i in range(2)])
            ybr = Rot([(sb("myb%d" % i, [128, 256], BF16), Buf()) for i in range(2)])
            ytr = Rot([(sb("myt%d" % i, [128, 2, 128], BF16), Buf()) for i in range(2)])
            CVv = CV.rearrange("(m p) f -> p m f", p=128)
            COv = CO.rearrange("(m p) f -> p m f", p=128)
            YCv = YC_T.rearrange("(g p) t -> p g t", p=128)
            orders = [[16, 17] + list(range(16)), [17, 16] + list(range(15, -1, -1))]
            def ld_mh(h_):
                M_ = mbufs.next()
                k.dma(SY, M_["q"][:], CQ_T[h_ * 128:(h_ + 1) * 128, :], reads=[fb], writes=[M_["b"]])
                k.dma(SY, M_["k"][:], CK_T[h_ * 128:(h_ + 1) * 128, :], reads=[fb], writes=[M_["b"]])
                k.dma(SY, M_["v"][:, :, 0:256], CVv[:, :, h_ * 256:(h_ + 1) * 256], reads=[fb], writes=[M_["b"]])
                k.dma(SY, M_["c"][:], COv[:, :, h_ * 256:(h_ + 1) * 256], reads=[fb], writes=[M_["b"]])
                k.op(DVE, lambda e: e.memset(M_["v"][:, :, 256:257], 1.0), writes=[M_["b"]])
                return M_
            nxt_mh = ld_mh(0)
            for h in range(4):
                MM = nxt_mh
                if h + 1 < 4:
                    nxt_mh = ld_mh(h + 1)
                qT, kT, Vp, coh, hb = MM["q"], MM["k"], MM["v"], MM["c"], MM["b"]
                for d_ in range(2):
                    k.op(DVE, lambda e: e.memset(Cst[d_][0][:], 0.0), writes=[Cst[d_][1]])
                    k.op(DVE, lambda e: e.memset(Cbf[d_][0][:], 0.0), writes=[Cbf[d_][1]])
                written = [False] * NTILE
                for step in range(NTILE):
                    recs = []
                    for d_ in range(2):
                        c = orders[d_][step]
                        r_ = dict(d=d_, c=c, dh=d_ * 4 + h, cs=slice(c * 128, (c + 1) * 128), want=((c < 16) or ctx_out), upd=(step < NTILE - 1))
                        recs.append(r_)
                        cs = r_["cs"]
                        if r_["want"]:
                            pSt, pSb = aux[1]
                            pS = pSt[:, d_ * 128:(d_ + 1) * 128]
                            k.op(PE, lambda e: e.matmul(pS, kT[:, cs], qT[:, cs], start=True, stop=True), reads=[hb], writes=[pSb])
                            r_["pS"], r_["pSb"] = pS, pSb
                        if r_["upd"]:
                            pT_, pTb_ = tb[d_]
                            k.op(PE, lambda e: e.transpose(pT_[:, 0:128], kT[:, cs], identB[:]), reads=[hb, cbb], writes=[pTb_])
                            r_["pT"], r_["pTb"] = pT_, pTb_
                    for r_ in recs:
                        d_, c, dh = r_["d"], r_["c"], r_["dh"]
                        if r_["want"]:
                            pS, pSb = r_["pS"], r_["pSb"]
                            sp, spb = spr.next()
                            k.op(DVE, lambda e: e.scalar_tensor_tensor(out=sp[:], in0=pS, scalar=KS[:, c, dh:dh + 1], in1=triF[d_],
                                                                       op0=ALU.mult, op1=ALU.mult), reads=[pSb, gb, cstb], writes=[spb])
                            r_["sp"], r_["spb"] = sp, spb
                        if r_["upd"]:
                            pT_, pTb_ = r_["pT"], r_["pTb"]
                            kt, ktb = ktr.next()
                            k.op(ACT, lambda e: e.activation(out=kt[:], in_=pT_[:, 0:128], func=AF.Copy, scale=KSD[:, c, dh:dh + 1]), reads=[pTb_, gb], writes=[ktb])
                            r_["kt"], r_["ktb"] = kt, ktb
                    for r_ in recs:
                        d_, c = r_["d"], r_["c"]
                        if r_["upd"]:
                            kt, ktb = r_["kt"], r_["ktb"]
                            pC, pCb = mm[2 + d_]
                            k.op(PE, lambda e: e.matmul(pC[:, 0:257], kt[:], Vp[:, c, :], start=True, stop=True), reads=[ktb, hb], writes=[pCb])
                            r_["pC"], r_["pCb"] = pC, pCb
                    for r_ in recs:
                        d_, c, dh = r_["d"], r_["c"], r_["dh"]
                        if r_["upd"]:
                            pC, pCb = r_["pC"], r_["pCb"]
                            k.op(DVE, lambda e: e.scalar_tensor_tensor(out=Cst[d_][0][:], in0=Cst[d_][0][:], scalar=DEC[:, c, dh:dh + 1], in1=pC[:, 0:257],
                                                                       op0=ALU.mult, op1=ALU.add), reads=[pCb, Cst[d_][1], gb], writes=[Cst[d_][1]])
                    for r_ in recs:
                        d_, c, cs = r_["d"], r_["c"], r_["cs"]
                        if r_["want"]:
                            sp, spb = r_["sp"], r_["spb"]
                            pN, pNb = mm[2 + d_]
                            k.op(PE, lambda e: e.matmul(pN[:, 0:257], sp[:], Vp[:, c, :], start=True, stop=False), reads=[spb, hb], writes=[pNb])
                            k.op(PE, lambda e: e.matmul(pN[:, 0:257], qT[:, cs], Cbf[d_][0][:], start=False, stop=True), reads=[hb, Cbf[d_][1]], writes=[pNb])
                            r_["pN"], r_["pNb"] = pN, pNb
                    for r_ in recs:
                        d_ = r_["d"]
                        if r_["upd"]:
                            k.op(ACT, lambda e: e.activation(out=Cbf[d_][0][:], in_=Cst[d_][0][:], func=AF.Copy), reads=[Cst[d_][1]], writes=[Cbf[d_][1]])
                    for r_ in recs:
                        d_, c, dh = r_["d"], r_["c"], r_["dh"]
                        if r_["want"]:
                            pN, pNb = r_["pN"], r_["pNb"]
                            s3, s3b = sc_r.next()
                            k.op(DVE, lambda e: e.tensor_tensor(out=s3[:, 0:1], in0=pN[:, 256:257], in1=AINV[:, c, dh:dh + 1], op=ALU.max), reads=[pNb, gb], writes=[s3b])
                            k.op(DVE, lambda e: e.scalar_tensor_tensor(out=s3[:, 2:3], in0=pN[:, 256:257], scalar=-1.0, in1=s3[:, 0:1], op0=ALU.mult, op1=ALU.max), reads=[pNb, s3b], writes=[s3b])
                            k.op(DVE, lambda e: e.reciprocal(out=s3[:, 1:2], in_=s3[:, 2:3]), reads=[s3b], writes=[s3b])
                            hdst = Hs[:, c, :]
                            if not written[c]:
                                k.op(ACT, lambda e: e.activation(out=hdst, in_=pN[:, 0:256], func=AF.Copy, scale=s3[:, 1:2]), reads=[pNb, s3b], writes=[Hb[c]])
                                written[c] = True
                            else:
                                k.op(DVE, lambda e: e.scalar_tensor_tensor(out=hdst, in0=pN[:, 0:256], scalar=s3[:, 1:2], in1=hdst, op0=ALU.mult, op1=ALU.add),
                                     reads=[pNb, s3b], writes=[Hb[c]])
                    yield
                ntl = NTILE if ctx_out else 16
                k.op(ACT, lambda e: e.activation(out=sg32[:, 0:ntl, :], in_=coh[:, 0:ntl, :], func=AF.Exp, scale=-1.0), reads=[hb], writes=[sgb32])
                k.op(ACT, lambda e: e.activation(out=sg32[:, 0:ntl, :], in_=sg32[:, 0:ntl, :], func=AF.Ln, bias=epsT[:, 1:2]), reads=[sgb32, epsb], writes=[sgb32])
                k.op(ACT, lambda e: e.activation(out=sg32[:, 0:ntl, :], in_=sg32[:, 0:ntl, :], func=AF.Exp, scale=-1.0), reads=[sgb32], writes=[sgb32])
                k.op(DVE, lambda e: e.memset(ss18[:], 0.0), writes=[ss18b])
                for c in range(ntl):
                    k.op(ACT, lambda e: e.activation(out=junk[:], in_=Hs[:, c, :], func=AF.Square, accum_out=ss18[:, c:c + 1]), reads=[Hb[c]], writes=[junkb, ss18b])
                rstd_of(ss18[:, 0:ntl], ss18b, 256)
                yield
                for c in range(ntl):
                    y, yb_ = yr.next()
                    k.op(DVE, lambda e: e.scalar_tensor_tensor(out=y[:], in0=Hs[:, c, :], scalar=ss18[:, c:c + 1], in1=gmn[:, h * 256:(h + 1) * 256], op0=ALU.mult, op1=ALU.mult),
                         reads=[Hb[c], ss18b, gb], writes=[yb_])
                    ybf, ybfb = ybr.next()
                    k.op(DVE, lambda e: e.tensor_tensor(out=ybf[:], in0=y[:], in1=sg32[:, c, :], op=ALU.mult), reads=[yb_, sgb32], writes=[ybfb])
                    pT_, pTb_ = tbr.next()
                    for g in range(2):
                        k.op(PE, lambda e: e.transpose(pT_[:, g * 128:(g + 1) * 128], ybf[:, g * 128:(g + 1) * 128], identB[:]), reads=[ybfb, cbb], writes=[pTb_])
                    yt, ytb = ytr.next()
                    evac(yt[:].rearrange("p a b -> p (a b)"), pT_[:, 0:256], [pTb_], [ytb])
                    k.dma(SY, YCv[:, 2 * h:2 * h + 2, c * 128:(c + 1) * 128], yt[:], reads=[ytb], writes=[])
                    if c % 2 == 1:
                        yield

        def phase_merge(l, fb, tgs):
            with contextlib.ExitStack() as les:
                def sb(name, shape, dt):
                    return les.enter_context(nc.sbuf_tensor(un(name), shape, dt))
                tend = tgs[-1][0] + tgs[-1][1]
                Y = [sb("mgY%d" % i, [128, 8, NT], BF16) for i in range(3)]
                yb_ = Buf()
                for i, src in enumerate((YA_T, YB_T, YC_T)):
                    k.dma(SY, Y[i][:, :, 0:tend], src.rearrange("(g p) t -> p g t", p=128)[:, :, 0:tend], reads=[fb], writes=[yb_])
                wbs = [(sb("mgw%d" % i, [128, 3, 8, 128], BF16), Buf()) for i in range(2)]
                gts = Rot([(sb("mgg%d" % i, [128, 3, NT], BF16), Buf()) for i in range(2)])
                tr_ = Rot([(sb("mgt%d" % i, [128, 512], F32), Buf()) for i in range(4)])
                mgr = Rot([(sb("mgm%d" % i, [128, NT], BF16), Buf()) for i in range(2)])
                GTv = GATE_T.rearrange("(i f p) t -> f p i t", i=3, p=128)

                def ld(f, slot):
                    k.dma(GQ, slot[0][:].rearrange("p a b c -> p (a b c)"), wbr_in[l, f], writes=[slot[1]])
                st = WStream(wbs, ld, 16)
                def prep_gate(f_):
                    gt_, gtb_ = gts.next()
                    k.dma(SY, gt_[:, :, 0:tend], GTv[f_][:, :, 0:tend], reads=[fb], writes=[gtb_])
                    k.op(ACT, lambda e: e.activation(out=gt_[:, :, 0:tend], in_=gt_[:, :, 0:tend], func=AF.Sigmoid), reads=[gtb_], writes=[gtb_])
                    return gt_, gtb_
                nxt_gate = prep_gate(0)
                for f in range(16):
                    w, wb_ = st.get(f)
                    gt, gtb = nxt_gate
                    if f + 1 < 16:
                        nxt_gate = prep_gate(f + 1)
                    sg, sgb = gt, gtb
                    mg, mgb = mgr.next()
                    for (t0, tn) in tgs:
                        ts_ = []
                        for i in range(3):
                            pt, ptb = mmr.next()
                            for kc in range(8):
                                k.op(PE, lambda e: e.matmul(pt[:, 0:tn], w[:, i, kc, :], Y[i][:, kc, t0:t0 + tn], start=(kc == 0), stop=(kc == 7)),
                                     reads=[wb_, yb_], writes=[ptb])
                            t, tb_ = tr_.next()
                            k.op(DVE, lambda e: e.tensor_tensor(out=t[:, 0:tn], in0=pt[:, 0:tn], in1=sg[:, i, t0:t0 + tn], op=ALU.mult), reads=[ptb, sgb], writes=[tb_])
                            ts_.append((t, tb_))
                        k.op(DVE, lambda e: e.tensor_tensor(out=ts_[0][0][:, 0:tn], in0=ts_[0][0][:, 0:tn], in1=ts_[1][0][:, 0:tn], op=ALU.add),
                             reads=[ts_[0][1], ts_[1][1]], writes=[ts_[0][1]])
                        k.op(DVE, lambda e: e.tensor_tensor(out=mg[:, t0:t0 + tn], in0=ts_[0][0][:, 0:tn], in1=ts_[2][0][:, 0:tn], op=ALU.add),
                             reads=[ts_[0][1], ts_[2][1]], writes=[mgb])
                    k.dma(SY, MG_T[f * 128:(f + 1) * 128, 0:tend], mg[:, 0:tend], reads=[mgb], writes=[])

        def phase_out(l, fb, tiles):
            with contextlib.ExitStack() as les:
                def sb(name, shape, dt):
                    return les.enter_context(nc.sbuf_tensor(un(name), shape, dt))
                tend = (tiles[-1] + 1) * 128
                k.dma(SY, k.BIG[:, :, 0:tend], MG_T.rearrange("(c p) t -> p c t", p=128)[:, :, 0:tend], reads=[fb], writes=BIGB)
                gt1 = sb("ogt", [128, 2, D], F32)
                gb = Buf()
                for v in range(2):
                    k.dma(SY, gt1[:, v, :], MODROW[l, v, 32 * 128:48 * 128].partition_broadcast(128), reads=[modb], writes=[gb])
                xr = Rot([(sb("ox%d" % i, [128, 512], F32), Buf()) for i in range(3)])
                tr_ = Rot([(sb("ot%d" % i, [128, 512], F32), Buf()) for i in range(3)])

                def ld(n, slot):
                    k.dma(GQ, slot[0][:].rearrange("p a b -> p (a b)"), wout_in[l, n], writes=[slot[1]])
                st = WStream(k.wslot, ld, 4)
                its = [(n_, m_) for n_ in range(4) for m_ in tiles]
                xld = {}
                nld = [0]

                def ensure_x(upto):
                    while nld[0] <= min(upto, len(its) - 1):
                        n_, m_ = its[nld[0]]
                        xs_, xsb_ = xr.next()
                        src_, srcb_ = x_src(l, 1, m_)
                        k.dma(SY, xs_[:], src_[:, n_ * 512:(n_ + 1) * 512], reads=(srcb_[n_] if srcb_ else []), writes=[xsb_])
                        xld[nld[0]] = (xs_, xsb_)
                        nld[0] += 1
                for n in range(4):
                    w, wb_ = st.get(n)
                    for mi_, m in enumerate(tiles):
                        v = 0 if m < 16 else 1
                        it_ = n * len(tiles) + mi_
                        ensure_x(it_ + 2)
                        xs, xsb = xld.pop(it_)
                        pt, ptb = mmr.next()
                        for kc in range(16):
                            k.op(PE, lambda e: e.matmul(pt[:, :], k.BIG[:, kc, m * 128:(m + 1) * 128], w[:, kc, :], start=(kc == 0), stop=(kc == 15)),
                                 reads=[wb_, BIGB[m][kc]], writes=[ptb])
                        t, tb_ = tr_.next()
                        k.op(DVE, lambda e: e.tensor_tensor(out=t[:], in0=pt[:, :], in1=gt1[:, v, n * 512:(n + 1) * 512], op=ALU.mult), reads=[ptb, gb], writes=[tb_])
                        k.op(DVE, lambda e: e.tensor_tensor(out=t[:], in0=t[:], in1=xs[:], op=ALU.add), reads=[tb_, xsb], writes=[tb_])
                        k.dma(SY, XL[m * 128:(m + 1) * 128, n * 512:(n + 1) * 512], t[:], reads=[tb_], writes=[XB[m][n]])

        def phase_moe(l, tiles, last, fb2):
            with contextlib.ExitStack() as les:
                def sb(name, shape, dt):
                    return les.enter_context(nc.sbuf_tensor(un(name), shape, dt))
                half = (len(tiles) + 1) // 2
                groups = [tiles[:half], tiles[half:]]
                ng = max(len(g) for g in groups)
                acc = sb("eacc", [128, ng, D], F32)
                h2g = sb("eh2", [128, 16, ng * 128], BF16)
                h2gb = Buf()
                les2 = contextlib.ExitStack()
                les2.__enter__()
                sb_outer = sb

                def sb(name, shape, dt):
                    return les2.enter_context(nc.sbuf_tensor(un(name), shape, dt))
                accb = [bufs(4) for _ in range(ng)]
                heT = sb("ehe", [128, 8, ng * 128], BF16)
                heb = Buf()
                wg_s = [(sb("ewg%d" % i, [128, 16, 128], BF16), Buf()) for i in range(2)]
                wu_s = [(sb("ewu%d" % i, [128, 16, 128], BF16), Buf()) for i in range(2)]
                wd_s = [(sb("ewd%d" % i, [128, 8, 512], BF16), Buf()) for i in range(2)]
                sgr = Rot([(sb("esg%d" % i, [128, 512], F32), Buf()) for i in range(2)])
                gb = Buf()
                for gi_, grp in enumerate(groups):
                    t0 = grp[0] * 128
                    ntok = len(grp) * 128
                    if gi_ > 0:
                        les2 = contextlib.ExitStack()
                        les2.__enter__()
                        wg_s = [(sb("ewg%d" % i, [128, 16, 128], BF16), Buf()) for i in range(2)]
                        wu_s = [(sb("ewu%d" % i, [128, 16, 128], BF16), Buf()) for i in range(2)]
                        wd_s = [(sb("ewd%d" % i, [128, 8, 512], BF16), Buf()) for i in range(2)]
                        heT = sb("ehe", [128, 8, ng * 128], BF16)
                        sgr = Rot([(sb("esg%d" % i, [128, 512], F32), Buf()) for i in range(2)])
                    k.dma(SY, h2g[:, :, 0:ntok], H2T.rearrange("(c p) t -> p c t", p=128)[:, :, t0:t0 + ntok], reads=[fb2], writes=[h2gb])
                    if ntok == 1152:
                        subs = [(0, 384), (384, 384), (768, 384)]
                    else:
                        subs = [(s_, min(512, ntok - s_)) for s_ in range(0, ntok, 512)]

                    def ldg(i, slot):
                        k.dma(GQ, slot[0][:].rearrange("p a b -> p (a b)"), weg_in[l, i // 8, i % 8], writes=[slot[1]])

                    def ldu(i, slot):
                        k.dma(GQ, slot[0][:].rearrange("p a b -> p (a b)"), weu_in[l, i // 8, i % 8], writes=[slot[1]])

                    def ldd(i, slot):
                        k.dma(GQ, slot[0][:].rearrange("p a b -> p (a b)"), wed_in[l, i // 4, i % 4], writes=[slot[1]])
                    sg_ = WStream(wg_s, ldg, NE * 8)
                    su_ = WStream(wu_s, ldu, NE * 8)
                    sd_ = WStream(wd_s, ldd, NE * 4)
                    for e_ in range(NE):
                        for j in range(8):
                            wg, wgb = sg_.get(e_ * 8 + j)
                            wu, wub = su_.get(e_ * 8 + j)
                            for (s0, sn) in subs:
                                pg, pgb = mmr.next()
                                pu, pub = mmr.next()
                                rb = h2gb
                                for kc in range(16):
                                    k.op(PE, lambda e: e.matmul(pg[:, 0:sn], wg[:, kc, :], h2g[:, kc, s0:s0 + sn], start=(kc == 0), stop=(kc == 15)),
                                         reads=[wgb, rb], writes=[pgb])
                                for kc in range(16):
                                    k.op(PE, lambda e: e.matmul(pu[:, 0:sn], wu[:, kc, :], h2g[:, kc, s0:s0 + sn], start=(kc == 0), stop=(kc == 15)),
                                         reads=[wub, rb], writes=[pub])
                                sg, sgb = sgr.next()
                                k.op(ACT, lambda e: e.activation(out=sg[:, 0:sn], in_=pg[:, 0:sn], func=AF.Silu), reads=[pgb], writes=[sgb])
                                k.op(DVE, lambda e: e.tensor_tensor(out=heT[:, j, s0:s0 + sn], in0=pu[:, 0:sn], in1=sg[:, 0:sn], op=ALU.mult),
                                     reads=[pub, sgb], writes=[heb])
                        for n in range(4):
                            wd, wdb = sd_.get(e_ * 4 + n)
                            for mi, m in enumerate(grp):
                                pt, ptb = mmr.next()
                                for jc in range(8):
                                    k.op(PE, lambda e: e.matmul(pt[:, :], heT[:, jc, mi * 128:(mi + 1) * 128], wd[:, jc, :], start=(jc == 0), stop=(jc == 7)),
                                         reads=[wdb, heb], writes=[ptb])
                                dst = acc[:, mi, n * 512:(n + 1) * 512]
                                if e_ == 0:
                                    k.op(DVE, lambda e: e.tensor_scalar(out=dst, in0=pt[:, :], scalar1=comb[:, m, e_:e_ + 1], scalar2=None, op0=ALU.mult),
                                         reads=[ptb, combb[m]], writes=[accb[mi][n]])
                                else:
                                    k.op(DVE, lambda e: e.scalar_tensor_tensor(out=dst, in0=pt[:, :], scalar=comb[:, m, e_:e_ + 1], in1=dst, op0=ALU.mult, op1=ALU.add),
                                         reads=[ptb, combb[m]], writes=[accb[mi][n]])
                    k.barrier()
                    les2.__exit__(None, None, None)
                    les3 = contextlib.ExitStack()
                    les3.__enter__()

                    def sb3(name, shape, dt):
                        return les3.enter_context(nc.sbuf_tensor(un(name), shape, dt))
                    gt2 = sb3("egt", [128, 2, D], F32)
                    gfin = sb3("egf", [128, D], F32)
                    k.xt_r = Rot([(sb3("ext%d" % i, [128, D], F32), Buf()) for i in range(3)])
                    k.xn_r = Rot([(sb3("exn%d" % i, [128, D], F32), Buf()) for i in range(1)])
                    for v in range(2):
                        k.dma(SY, gt2[:, v, :], MODROW[l, v, 80 * 128:96 * 128].partition_broadcast(128), reads=[modb], writes=[gb])
                    k.dma(SY, gfin[:], gfin_in.partition_broadcast(128), writes=[gb])
                    rld = {}

                    def issue_r(m_):
                        xt_, xtb_ = k.xt_r.next()
                        k.dma(SY, xt_[:], XL[m_ * 128:(m_ + 1) * 128, :], reads=XB[m_], writes=[xtb_])
                        rld[m_] = (xt_, xtb_)
                    issue_r(grp[0])
                    for mi, m in enumerate(grp):
                        v = 0 if m < 16 else 1
                        if mi + 1 < len(grp):
                            issue_r(grp[mi + 1])
                        xt, xtb = rld.pop(m)
                        k.op(DVE, lambda e: e.tensor_tensor(out=acc[:, mi, :], in0=acc[:, mi, :], in1=gt2[:, v, :], op=ALU.mult), reads=[accb[mi], gb], writes=[accb[mi]])
                        k.op(DVE, lambda e: e.tensor_tensor(out=xt[:], in0=xt[:], in1=acc[:, mi, :], op=ALU.add), reads=[xtb, accb[mi]], writes=[xtb])
                        if not last:
                            k.dma(SY, XL[m * 128:(m + 1) * 128, :], xt[:], reads=[xtb], writes=XB[m])
                        else:
                            xn, xnb = k.xn_r.next()
                            ss, ssb = sc_r.next()
                            k.op(DVE, lambda e: e.memset(ss[:, 0:1], 0.0), writes=[ssb])
                            k.op(ACT, lambda e: e.activation(out=xn[:], in_=xt[:], func=AF.Square, accum_out=ss[:, 0:1]), reads=[xtb], writes=[xnb, ssb])
                            rstd_of(ss[:, 0:1], ssb, D)
                            k.op(DVE, lambda e: e.scalar_tensor_tensor(out=xn[:], in0=xt[:], scalar=ss[:, 0:1], in1=gfin[:], op0=ALU.mult, op1=ALU.mult),
                                 reads=[xtb, ssb, gb], writes=[xnb])
                            k.dma(SY, y_out[m * 128:(m + 1) * 128, :], xn[:], reads=[xnb], writes=[])
                    k.barrier()
                    les3.__exit__(None, None, None)

        k.rt_r = Rot([(k.sb("rtR%d" % i, [128, 160], F32), Buf()) for i in range(2)])
        ALLT = list(range(NTILE))
        LAT = list(range(16))
        for l in range(n_layers):
            last = (l == L - 1)
            k.barrier()
            with Scope() as S:
                alloc_big(S)
                alloc_wslot(S)
                alloc_st(S)
                with Scope() as S2:
                    alloc_x(S2)
                    phase_norm(l, 1, ALLT)
                    if "HD" in dbg and l == 0:
                        k.dma(SY, HD.rearrange("(c p) t -> p c t", p=128), k.BIG[:], reads=BIGB, writes=[])
                    k.barrier()
                if stop == "norm1":
                    break
                gi = phase_inproj(l, S, last)
                for tok_ in gi:
                    if tok_ == "AUAV":
                        break
                fb_a = fence_bufs()
                with Scope() as S3:
                    gs_ = phase_sgu(l, fb_a, LAT if last else ALLT, S3.sb, GQ)
                    live = [True, True]
                    cnt_ = 0
                    while live[0] or live[1]:
                        if live[0]:
                            try:
                                next(gi)
                            except StopIteration:
                                live[0] = False
                        cnt_ += 1
                        if live[1] and (cnt_ % 3 == 0 or not live[0]):
                            try:
                                next(gs_)
                            except StopIteration:
                                live[1] = False
                    k.barrier()
            fb = fence_bufs()
            if stop in ("inproj", "sgu"):
                break
            k.barrier()
            phase_rope(fb)
            fb = fence_bufs()
            if stop == "na":
                break
            k.barrier()
            with Scope() as S:
                gens = [phase_na(l, fb, not last, S.sb), phase_mlstm(l, fb, not last, S.sb)]
                while gens:
                    for g_ in list(gens):
                        try:
                            next(g_)
                        except StopIteration:
                            gens.remove(g_)
                k.barrier()
            fb = fence_bufs()
            if stop == "mlstm":
                break
            k.barrier()
            phase_merge(l, fb, TG[:4] if last else TG)
            fb = fence_bufs()
            if stop == "merge":
                break
            k.barrier()
            with Scope() as S:
                alloc_big(S)
                alloc_wslot(S)
                phase_out(l, fb, LAT if last else ALLT)
            if stop == "out":
                break
            k.barrier()
            with Scope() as S:
                alloc_x(S)
                k.hf_r = Rot([(S.sb("hf%d" % i, [128, 16, 128], F32), bufs(16)) for i in range(2)])
                k.h2s_r = Rot([(S.sb("h2s%d" % i, [128, 16, 128], BF16), bufs(16)) for i in range(2)])
                phase_norm(l, 2, LAT if last else ALLT, router=True)
            fb2 = fence_bufs()
            if stop == "norm2":
                break
            k.barrier()
            phase_moe(l, LAT if last else ALLT, last, fb2)
        k.barrier()
    return nc


def _c(a):
    return np.ascontiguousarray(a, dtype=np.float32)


def prep_shared(inp):
    global _NA_IDX
    W = {}
    w_ada = inp["w_ada"]
    W["wada"] = _c(w_ada.reshape(L, 16, 128, 24, 512).transpose(0, 3, 2, 1, 4)).reshape(L, 24, 128, 16 * 512)
    W["bada"] = _c(inp["b_ada"].reshape(L, 96, 128).transpose(2, 0, 1))
    W["g1"] = _c(inp["g_norm1"].reshape(L, 16, 128).transpose(2, 0, 1))
    W["g2"] = _c(inp["g_norm2"].reshape(L, 16, 128).transpose(2, 0, 1))
    w_in = inp["w_in"]
    W["win"] = _c(w_in[:, :, 0:8192].reshape(L, 16, 128, 16, 512).transpose(0, 3, 2, 1, 4)).reshape(L, 16, 128, 16 * 512)
    W["wcg"] = _c(w_in[:, :, 8192:8208].reshape(L, 16, 128, 16).transpose(0, 2, 1, 3)).reshape(L, 128, 256)
    W["wgate"] = _c(w_in[:, :, 8208:14352].reshape(L, 16, 128, 12, 512).transpose(0, 3, 2, 1, 4)).reshape(L, 12, 128, 16 * 512)
    W["gsgu"] = _c(inp["g_sgu"])
    W["wsp"] = _c(inp["w_spatial"].transpose(0, 3, 1, 2)).reshape(L, 128, 1024)
    W["bsp"] = _c(inp["b_spatial"].reshape(L, 1024))
    if _NA_IDX is None:
        _NA_IDX = na_index_tables()
    rpb = inp["na_rpb"].reshape(L, 8, 465)
    rpbx = np.concatenate([rpb, np.full((L, 8, 1), -1e30, np.float32)], axis=2)
    tab = rpbx[:, :, _NA_IDX]
    W["natab"] = _c(tab.transpose(0, 1, 3, 2, 4, 5)).reshape(L, 8, 128, 5 * 5 * 128)
    t = np.arange(SEQ)
    pr, pc = (t // 64).astype(np.float64), (t % 64).astype(np.float64)
    inv = 10000.0 ** (-np.arange(32, dtype=np.float64) / 32)
    d = np.arange(128)
    pos = np.where(d[:, None] < 64, pr[None, :], pc[None, :])
    ang = pos * inv[d % 32][:, None]
    cos = np.cos(ang)
    sins = np.sin(ang) * np.where((d % 64) < 32, -1.0, 1.0)[:, None]
    sc = 128 ** -0.5
    W["rope"] = _c(np.stack([cos * sc, sins * sc, cos, sins], axis=1)).reshape(128, 4 * SEQ)
    ident = np.eye(128)
    s_i, t_i = np.meshgrid(np.arange(128), np.arange(128), indexing="ij")
    trif = (s_i <= t_i).astype(np.float64)
    trib = (s_i >= t_i).astype(np.float64)
    perm = np.zeros((128, 128))
    for m in range(128):
        partner = m + 32 if (m % 64) < 32 else m - 32
        perm[partner, m] = 1.0
    W["consts"] = _c(np.stack([ident, trif, trib, np.ones((128, 128)), perm], axis=1)).reshape(128, 5 * 128)
    W["bmg"] = _c(inp["b_mgate"].reshape(L, 16))
    W["gmn"] = _c(inp["g_mnorm"])
    W["wbr"] = _c(inp["w_branch"].reshape(L, 3, 8, 128, 16, 128).transpose(0, 4, 3, 1, 2, 5)).reshape(L, 16, 128, 3 * 8 * 128)
    W["wout"] = _c(inp["w_out"].reshape(L, 16, 128, 4, 512).transpose(0, 3, 2, 1, 4)).reshape(L, 4, 128, 16 * 512)
    W["wr"] = _c(inp["w_router"].reshape(16, 128, 16).transpose(1, 0, 2)).reshape(128, 256)
    W["br"] = _c(inp["b_router"])
    W["weg"] = _c(inp["w_e_gate"].reshape(L, NE, 16, 128, 8, 128).transpose(0, 1, 4, 3, 2, 5)).reshape(L, NE, 8, 128, 16 * 128)
    W["weu"] = _c(inp["w_e_up"].reshape(L, NE, 16, 128, 8, 128).transpose(0, 1, 4, 3, 2, 5)).reshape(L, NE, 8, 128, 16 * 128)
    W["wed"] = _c(inp["w_e_down"].reshape(L, NE, 8, 128, 4, 512).transpose(0, 1, 4, 3, 2, 5)).reshape(L, NE, 4, 128, 8 * 512)
    W["gfin"] = _c(inp["g_final"])
    return W


def prep_core(inp, b):
    m = {}
    m["x"] = _c(inp["x"][b])
    m["ctx"] = _c(inp["ctx"][b])
    cv = np.stack([inp["c"][b].reshape(16, 128), inp["c_ctx"].reshape(16, 128)], axis=-1)
    m["cvec"] = _c(cv.transpose(1, 0, 2))
    return m


def kernel(**inputs):
    inp = {k_: np.asarray(v) for k_, v in inputs.items()}
    W = prep_shared(inp)
    nc = build()
    n = 8
    in_maps = []
    for b in range(n):
        m = dict(W)
        m.update(prep_core(inp, b))
        in_maps.append(m)
    res = run_bass_kernel_spmd(nc, in_maps, core_ids=list(range(n)))
    return np.stack([np.asarray(r["y"], dtype=np.float32) for r in res.results], axis=0)
```

```python
import contextlib
import numpy as np
import ml_dtypes
import concourse.bass as bass
import concourse.mybir as mybir
from concourse.bass_utils import run_bass_kernel_spmd

F32 = mybir.dt.float32
BF16 = mybir.dt.bfloat16
AF = mybir.ActivationFunctionType
ALU = mybir.AluOpType

D = 2048
SEQ = 2048
CTX = 256
NT = SEQ + CTX
NTILE = NT // 128
L = 2
EPS = 1e-6
NE = 16
GELU_C = 1.5957691216057308
NA_SCALE = 128 ** -0.5


_UN = [0]


def un(name):
    _UN[0] += 1
    return "t%d_%s" % (_UN[0], name)


class Sem:
    __slots__ = ("h",)

    def __init__(self, h):
        self.h = h


class Buf:
    __slots__ = ("w", "r", "psum")

    def __init__(self, psum=False):
        self.w = {}
        self.r = {}
        self.psum = psum


def bufs(n):
    return [Buf() for _ in range(n)]


def flat(x):
    if isinstance(x, Buf):
        return [x]
    out = []
    for y in x:
        if isinstance(y, Buf):
            out.append(y)
        else:
            out.extend(flat(y))
    return out


class Eng:
    def __init__(self, k, eng, is_pe=False):
        self.k = k
        self.eng = eng
        self.is_pe = is_pe
        self.sem = k.new_sem()
        self.cnt = 0
        self.waited = {}


class DQ:
    def __init__(self, k, eng, nsem, waited=None):
        self.k = k
        self.eng = eng
        self.sems = [[k.new_sem(), 0] for _ in range(nsem)]
        self.i = 0
        self.waited = {} if waited is None else waited


class Rot:
    def __init__(self, items):
        self.items = items
        self.i = 0

    def next(self):
        it = self.items[self.i % len(self.items)]
        self.i += 1
        return it


class K:
    def __init__(self, nc, es):
        self.nc = nc
        self.es = es
        self.nsem = 0
        self.PE = Eng(self, nc.tensor, is_pe=True)
        self.ACT = Eng(self, nc.scalar)
        self.DVE = Eng(self, nc.vector)
        self.POOL = Eng(self, nc.gpsimd)
        self.SY = DQ(self, nc.sync, 24)
        self.GQ = DQ(self, nc.gpsimd, 24, waited=self.POOL.waited)
        self.evi = 0

    def new_sem(self):
        self.nsem += 1
        return Sem(self.es.enter_context(self.nc.semaphore("s%d" % self.nsem)))

    def sb(self, name, shape, dt):
        return self.es.enter_context(self.nc.sbuf_tensor(un(name), shape, dt))

    def ps(self, name, shape, dt):
        return self.es.enter_context(self.nc.psum_tensor(un(name), shape, dt))

    @staticmethod
    def _deps(reads, writes):
        d = {}
        for b in reads:
            for s, v in b.w.items():
                if d.get(s, 0) < v:
                    d[s] = v
            if b.psum:
                for s, v in b.r.items():
                    if d.get(s, 0) < v:
                        d[s] = v
        for b in writes:
            for s, v in b.w.items():
                if d.get(s, 0) < v:
                    d[s] = v
            for s, v in b.r.items():
                if d.get(s, 0) < v:
                    d[s] = v
        return d

    @staticmethod
    def _wait(E, d):
        for s, v in d.items():
            if E.waited.get(s, 0) >= v:
                continue
            E.eng.wait_ge(s.h, v)
            E.waited[s] = v

    @staticmethod
    def _record(reads, writes, s, v):
        for b in reads:
            if b.r.get(s, 0) < v:
                b.r[s] = v
        for b in writes:
            b.w = {s: v}
            b.r = {}

    def op(self, E, fn, reads=(), writes=()):
        reads = flat(reads)
        writes = flat(writes)
        d = self._deps(reads, writes)
        if E.is_pe:
            d.pop(E.sem, None)
        self._wait(E, d)
        ins = fn(E.eng)
        if E.cnt >= 60000:
            E.sem = self.new_sem()
            E.cnt = 0
        E.cnt += 1
        ins.then_inc(E.sem.h, 1)
        self._record(reads, writes, E.sem, E.cnt)

    def dma(self, Q, out, in_, reads=(), writes=()):
        reads = flat(reads)
        writes = flat(writes)
        slot = Q.sems[Q.i % len(Q.sems)]
        Q.i += 1
        d = self._deps(reads, writes)
        if slot[1] > 0 and d.get(slot[0], 0) < slot[1]:
            d[slot[0]] = slot[1]
        self._wait(Q, d)
        Q.eng.dma_start(out=out, in_=in_).then_inc(slot[0].h, 16)
        slot[1] += 16
        self._record(reads, writes, slot[0], slot[1])

    def barrier(self):
        toks = {}
        for E in (self.PE, self.ACT, self.DVE):
            if E.cnt > 0:
                toks[E.sem] = E.cnt
        for Q in (self.SY, self.GQ):
            for s, c in Q.sems:
                if c > 0:
                    toks[s] = c
        for E in (self.PE, self.ACT, self.DVE, self.SY, self.GQ):
            self._wait(E, dict(toks))

    def evac_eng(self):
        self.evi += 1
        return self.ACT if (self.evi & 1) else self.DVE


class WStream:
    def __init__(self, slots, loader, total):
        self.slots = slots
        self.loader = loader
        self.total = total
        self.nxt = 0

    def get(self, i):
        lim = min(i + len(self.slots) - 1, self.total - 1)
        while self.nxt <= lim:
            self.loader(self.nxt, self.slots[self.nxt % len(self.slots)])
            self.nxt += 1
        return self.slots[i % len(self.slots)]


def na_tiles(p):
    lo = min(max(2 * p - 4, 0), 24)
    hi = min(max(2 * p + 1 - 4, 0), 24) + 7
    return list(range(lo // 2, hi // 2 + 1))


NA_VAR_P = [0, 1, 2, 14, 15]


def na_var(p):
    if p < 2:
        return p
    if p > 13:
        return p - 11
    return 2


def na_index_tables():
    idx = np.full((5, 128, 5, 128), 465, np.int64)
    for vi, p in enumerate(NA_VAR_P):
        tiles = na_tiles(p)
        for si, t in enumerate(tiles):
            for kp in range(128):
                kr = 2 * t + kp // 64
                kc = kp % 64
                for q in range(128):
                    r = 2 * p + q // 64
                    cq = q % 64
                    rs = min(max(r - 4, 0), 24)
                    cs = min(max(cq - 8, 0), 48)
                    if rs <= kr < rs + 8 and cs <= kc < cs + 16:
                        ri = kr - r + 7
                        ci = min(max(kc - cq, -15), 15) + 15
                        idx[vi, kp, si, q] = ri * 31 + ci
    return idx


_NA_IDX = None


def build(n_layers=L, debug=(), stop=None, skip=()):
    nc = bass.Bass("TRN2", target_bir_lowering=False)
    es = contextlib.ExitStack()
    dbg = set(debug)

    def din(name, shape, dt=F32):
        if name in skip:
            return None
        return nc.dram_tensor(name, list(shape), dt, kind="ExternalInput").ap()

    def dscr(name, shape, dt):
        kind = "ExternalOutput" if name in dbg else "Internal"
        return nc.dram_tensor(name, list(shape), dt, kind=kind).ap()

    x_in = din("x", [SEQ, D])
    ctx_in = din("ctx", [CTX, D])
    cvec_in = din("cvec", [128, 16, 2])
    wada_in = din("wada", [L, 24, 128, 16 * 512])
    bada_in = din("bada", [128, L, 96])
    g1_in = din("g1", [128, L, 16])
    g2_in = din("g2", [128, L, 16])
    win_in = din("win", [L, 16, 128, 16 * 512])
    wcg_in = din("wcg", [L, 128, 16 * 16])
    wgate_in = din("wgate", [L, 12, 128, 16 * 512])
    gsgu_in = din("gsgu", [L, 1024])
    wsp_in = din("wsp", [L, 128, 8 * 128])
    bsp_in = din("bsp", [L, 1024])
    natab_in = din("natab", [L, 8, 128, 5 * 5 * 128])
    rope_in = din("rope", [128, 4 * SEQ])
    consts_in = din("consts", [128, 5 * 128])
    bmg_in = din("bmg", [L, 16])
    gmn_in = din("gmn", [L, 1024])
    wbr_in = din("wbr", [L, 16, 128, 3 * 8 * 128])
    wout_in = din("wout", [L, 4, 128, 16 * 512])
    wr_in = din("wr", [128, 16 * 16])
    br_in = din("br", [16])
    weg_in = din("weg", [L, NE, 8, 128, 16 * 128])
    weu_in = din("weu", [L, NE, 8, 128, 16 * 128])
    wed_in = din("wed", [L, NE, 4, 128, 8 * 512])
    gfin_in = din("gfin", [D])
    y_out = nc.dram_tensor("y", [SEQ, D], F32, kind="ExternalOutput").ap()

    XL = dscr("XL", [NT, D], F32)
    AU_T = dscr("AU_T", [1024, NT], BF16)
    AV = dscr("AV", [NT, 1024], BF16)
    BQ_T = dscr("BQ_T", [1024, NT], BF16)
    BK_T = dscr("BK_T", [1024, NT], BF16)
    BV = dscr("BV", [NT, 1024], BF16)
    CQ_T = dscr("CQ_T", [512, NT], BF16)
    CK_T = dscr("CK_T", [512, NT], BF16)
    CV = dscr("CV", [NT, 1024], BF16)
    CO = dscr("CO", [NT, 1024], BF16)
    CG = dscr("CG", [NT, 16], F32)
    GATE_T = dscr("GATE_T", [6144, NT], BF16)
    YA_T = dscr("YA_T", [1024, NT], BF16)
    YB_T = dscr("YB_T", [1024, NT], BF16)
    YC_T = dscr("YC_T", [1024, NT], BF16)
    MG_T = dscr("MG_T", [D, NT], BF16)
    MODROW = dscr("MODROW", [L, 2, 96 * 128], F32)
    HD = dscr("HD", [D, NT], BF16)
    CMB = dscr("CMB", [NT, 16], F32)
    H2T = dscr("H2T", [D, NT], BF16)

    with es:
        k = K(nc, es)
        PE, ACT, DVE, SY, GQ = k.PE, k.ACT, k.DVE, k.SY, k.GQ

        mm = [(k.ps("mm%d" % i, [128, 512], F32), Buf(psum=True)) for i in range(4)]
        aux = [(k.ps("aux%d" % i, [128, 512], F32), Buf(psum=True)) for i in range(2)]
        tb = [(k.ps("tb%d" % i, [128, 1024], BF16), Buf(psum=True)) for i in range(2)]
        mmr, auxr, tbr = Rot(mm), Rot(aux), Rot(tb)

        BIGB = [bufs(16) for _ in range(NTILE)]

        class Scope:
            def __init__(self):
                self.es = contextlib.ExitStack()

            def __enter__(self):
                self.es.__enter__()
                return self

            def __exit__(self, *a):
                return self.es.__exit__(*a)

            def sb(self, name, shape, dt):
                return self.es.enter_context(nc.sbuf_tensor(un(name), shape, dt))

        def alloc_big(S):
            k.BIG = S.sb("BIG", [128, 16, NT], BF16)

        def alloc_wslot(S):
            k.wslot = [(S.sb("wslot%d" % i, [128, 16, 512], BF16), Buf()) for i in range(2)]

        def alloc_x(S):
            k.xt_r = Rot([(S.sb("xt%d" % i, [128, D], F32), Buf()) for i in range(2)])
            k.xn_r = Rot([(S.sb("xn%d" % i, [128, D], F32), Buf()) for i in range(2)])
        cst = k.sb("cst", [128, 5, 128], F32)
        cstb = Buf()
        identF = cst[:, 0, :]
        triF = [cst[:, 1, :], cst[:, 2, :]]
        onesF = cst[:, 3, :]
        identB = k.sb("identB", [128, 128], BF16)
        onesB = k.sb("onesB", [128, 128], BF16)
        permB = k.sb("permB", [128, 128], BF16)
        cbb = Buf()
        modF = k.sb("modF", [128, L, 96, 2], F32)
        modb = Buf()
        bada = k.sb("bada", [128, L, 96], F32)
        g1s = k.sb("g1s", [128, L, 16], F32)
        g2s = k.sb("g2s", [128, L, 16], F32)
        smallb = Buf()
        Gp = k.sb("Gp", [128, 16, 2], F32)
        Shp = k.sb("Shp", [128, 16, 2], F32)
        gpb = Buf()
        sc_r = Rot([(k.sb("sc%d" % i, [128, 8], F32), Buf()) for i in range(4)])

        k.dma(SY, cst[:].rearrange("p a b -> p (a b)"), consts_in[:, :], writes=[cstb])
        k.op(ACT, lambda e: e.activation(out=identB[:], in_=cst[:, 0, :], func=AF.Copy), reads=[cstb], writes=[cbb])
        k.op(ACT, lambda e: e.activation(out=onesB[:], in_=cst[:, 3, :], func=AF.Copy), reads=[cstb], writes=[cbb])
        k.op(ACT, lambda e: e.activation(out=permB[:], in_=cst[:, 4, :], func=AF.Copy), reads=[cstb], writes=[cbb])
        k.dma(SY, bada[:].rearrange("p a b -> p (a b)"), bada_in.rearrange("p a b -> p (a b)"), writes=[smallb])
        k.dma(SY, g1s[:].rearrange("p a b -> p (a b)"), g1_in.rearrange("p a b -> p (a b)"), writes=[smallb])
        k.dma(SY, g2s[:].rearrange("p a b -> p (a b)"), g2_in.rearrange("p a b -> p (a b)"), writes=[smallb])

        def evac(out, in_, reads, writes, E=None):
            E = E or k.evac_eng()
            if E is ACT:
                k.op(ACT, lambda e: e.activation(out=out, in_=in_, func=AF.Copy), reads=reads, writes=writes)
            else:
                k.op(DVE, lambda e: e.tensor_copy(out=out, in_=in_), reads=reads, writes=writes)

        cv = k.sb("cv", [128, 16, 2], F32)
        cvs = k.sb("cvs", [128, 16, 2], BF16)
        cvb = Buf()
        k.dma(SY, cv[:].rearrange("p a b -> p (a b)"), cvec_in.rearrange("p a b -> p (a b)"), writes=[cvb])
        k.op(ACT, lambda e: e.activation(out=cvs[:], in_=cv[:], func=AF.Silu), reads=[cvb], writes=[cvb])
        S0 = Scope()
        S0.__enter__()
        alloc_wslot(S0)
        alloc_x(S0)
        for l in range(n_layers):
            def ld_ada(i, slot, l=l):
                k.dma(GQ, slot[0][:].rearrange("p a b -> p (a b)"), wada_in[l, i], writes=[slot[1]])
            st = WStream(k.wslot, ld_ada, 24)
            pst, psb = auxr.next()
            for jb in range(24):
                w, wb_ = st.get(jb)
                for jj in range(4):
                    j = jb * 4 + jj
                    for kc in range(16):
                        k.op(PE, lambda e: e.matmul(pst[:, 2 * j:2 * j + 2], w[:, kc, jj * 128:(jj + 1) * 128], cvs[:, kc, :],
                                                    start=(kc == 0), stop=(kc == 15)),
                             reads=[wb_, cvb], writes=[psb])
            for v in range(2):
                k.op(DVE, lambda e: e.tensor_tensor(out=modF[:, l, :, v], in0=pst[:, 0:192].rearrange("p (j v) -> p j v", v=2)[:, :, v], in1=bada[:, l, :], op=ALU.add),
                     reads=[psb, smallb], writes=[modb])
            for v in range(2):
                pt, ptb = auxr.next()
                k.op(PE, lambda e: e.transpose(pt[0:96, 0:128], modF[:, l, :, v], identF), reads=[modb, cstb], writes=[ptb])
                mr, mrb = k.xn_r.next()
                evac(mr[0:96, 0:128], pt[0:96, 0:128], [ptb], [mrb])
                k.dma(SY, MODROW[l, v].rearrange("(j p) -> j p", p=128), mr[0:96, 0:128], reads=[mrb], writes=[modb])
        k.barrier()
        S0.__exit__(None, None, None)

        def x_src(l, which, m):
            if l == 0 and which == 1:
                return (x_in[m * 128:(m + 1) * 128, :] if m < 16 else ctx_in[(m - 16) * 128:(m - 15) * 128, :]), []
            return XL[m * 128:(m + 1) * 128, :], XB[m]

        XB = [bufs(4) for _ in range(NTILE)]
        comb = k.sb("comb", [128, NTILE, 16], F32)
        combb = bufs(NTILE)
        wr_s = k.sb("wr_s", [128, 16, 16], F32)
        br_s = k.sb("br_s", [128, 16], F32)
        k.dma(SY, wr_s[:].rearrange("p a b -> p (a b)"), wr_in[:, :], writes=[smallb])
        k.dma(SY, br_s[:], br_in.partition_broadcast(128), writes=[smallb])

        epsT = k.sb("epsT", [128, 2], F32)
        epsb = Buf()
        k.op(DVE, lambda e: e.memset(epsT[:, 0:1], EPS), writes=[epsb])
        k.op(DVE, lambda e: e.memset(epsT[:, 1:2], 1.0), writes=[epsb])

        def rstd_of(ssq, ssb, n):
            k.op(ACT, lambda e: e.activation(out=ssq, in_=ssq, func=AF.Ln, scale=1.0 / n, bias=epsT[:, 0:1]), reads=[ssb, epsb], writes=[ssb])
            k.op(ACT, lambda e: e.activation(out=ssq, in_=ssq, func=AF.Exp, scale=-0.5), reads=[ssb], writes=[ssb])

        def phase_norm(l, which, tiles, router=False):
            gsrc = g1s if which == 1 else g2s
            jsh, jsc = (0, 16) if which == 1 else (48, 64)
            for v in range(2):
                k.op(DVE, lambda e: e.scalar_tensor_tensor(out=Gp[:, :, v], in0=modF[:, l, jsc:jsc + 16, v], scalar=1.0,
                                                           in1=gsrc[:, l, :], op0=ALU.add, op1=ALU.mult),
                     reads=[modb, smallb], writes=[gpb])
                k.op(DVE, lambda e: e.tensor_copy(out=Shp[:, :, v], in_=modF[:, l, jsh:jsh + 16, v]), reads=[modb], writes=[gpb])
            pend_ld = {}

            def issue_ld(m_):
                xt_, xtb_ = k.xt_r.next()
                src_, srcb_ = x_src(l, which, m_)
                k.dma(SY, xt_[:], src_, reads=srcb_, writes=[xtb_])
                pend_ld[m_] = (xt_, xtb_)
            issue_ld(tiles[0])
            deferred = []
            for idx_, m in enumerate(tiles):
                v = 0 if m < 16 else 1
                if idx_ + 1 < len(tiles):
                    issue_ld(tiles[idx_ + 1])
                xt, xtb = pend_ld.pop(m)
                xn, xnb = k.xn_r.next()
                ss, ssb = sc_r.next()
                k.op(DVE, lambda e: e.memset(ss[:, 0:1], 0.0), writes=[ssb])
                k.op(ACT, lambda e: e.activation(out=xn[:], in_=xt[:], func=AF.Square, accum_out=ss[:, 0:1]),
                     reads=[xtb], writes=[xnb, ssb])
                rstd_of(ss[:, 0:1], ssb, D)
                if router:
                    k.op(ACT, lambda e: e.activation(out=xn[:], in_=xt[:], func=AF.Copy, scale=ss[:, 0:1]),
                         reads=[xtb, ssb], writes=[xnb])
                else:
                    k.op(DVE, lambda e: e.tensor_scalar(out=xn[:], in0=xt[:], scalar1=ss[:, 0:1], scalar2=None, op0=ALU.mult),
                         reads=[xtb, ssb], writes=[xnb])
                if router:
                    hf = k.hf_r.next()
                    while deferred:
                        deferred.pop(0)()
                for q4 in range(4):
                    pt, ptb = auxr.next()
                    for i in range(4):
                        kc = q4 * 4 + i
                        k.op(PE, lambda e: e.transpose(pt[:, i * 128:(i + 1) * 128], xn[:, kc * 128:(kc + 1) * 128], identF),
                             reads=[xnb, cstb], writes=[ptb])
                    for i in range(4):
                        kc = q4 * 4 + i
                        if router:
                            if i == 0 and q4 == 0:
                                h2s, h2b = k.h2s_r.next()
                            dst = h2s[:, kc, :]
                            dstb = h2b[kc]
                        else:
                            dst = k.BIG[:, kc, m * 128:(m + 1) * 128]
                            dstb = BIGB[m][kc]
                        if router:
                            k.op(DVE, lambda e: e.tensor_scalar(out=hf[0][:, kc, :], in0=pt[:, i * 128:(i + 1) * 128],
                                                                scalar1=Gp[:, kc, v:v + 1], scalar2=Shp[:, kc, v:v + 1],
                                                                op0=ALU.mult, op1=ALU.add),
                                 reads=[ptb, gpb], writes=[hf[1][kc]])
                            k.op(ACT, lambda e: e.activation(out=dst, in_=hf[0][:, kc, :], func=AF.Copy),
                                 reads=[hf[1][kc]], writes=[dstb])
                        else:
                            E = ACT if (q4 & 1) == 0 else DVE
                            if E is ACT:
                                k.op(ACT, lambda e: e.activation(out=dst, in_=pt[:, i * 128:(i + 1) * 128], func=AF.Identity,
                                                                 scale=Gp[:, kc, v:v + 1], bias=Shp[:, kc, v:v + 1]),
                                     reads=[ptb, gpb], writes=[dstb])
                            else:
                                k.op(DVE, lambda e: e.tensor_scalar(out=dst, in0=pt[:, i * 128:(i + 1) * 128],
                                                                    scalar1=Gp[:, kc, v:v + 1], scalar2=Shp[:, kc, v:v + 1],
                                                                    op0=ALU.mult, op1=ALU.add),
                                     reads=[ptb, gpb], writes=[dstb])
                if router:
                    def back(m=m, hf=hf, h2s=h2s, h2b=h2b):
                        pr, prb = mmr.next()
                        for kc in range(16):
                            k.op(PE, lambda e: e.matmul(pr[:, 0:16], hf[0][:, kc, :], wr_s[:, kc, :], start=(kc == 0), stop=(kc == 15)),
                                 reads=[hf[1][kc], smallb], writes=[prb])
                        route(m, pr, prb)
                        k.dma(SY, H2T.rearrange("(c p) t -> p c t", p=128)[:, :, m * 128:(m + 1) * 128], h2s[:], reads=[h2b], writes=[])
                    deferred.append(back)
            while deferred:
                deferred.pop(0)()

        def route(m, pr, prb):
            R, Rb = k.rt_r.next()
            k.op(ACT, lambda e: e.activation(out=R[:, 0:16], in_=pr[:, 0:16], func=AF.Exp, scale=-1.0), reads=[prb], writes=[Rb])
            k.op(DVE, lambda e: e.tensor_scalar(out=R[:, 0:16], in0=R[:, 0:16], scalar1=1.0, scalar2=None, op0=ALU.add), reads=[Rb], writes=[Rb])
            k.op(DVE, lambda e: e.reciprocal(out=R[:, 0:16], in_=R[:, 0:16]), reads=[Rb], writes=[Rb])
            k.op(DVE, lambda e: e.tensor_tensor(out=R[:, 16:32], in0=R[:, 0:16], in1=br_s[:], op=ALU.add), reads=[Rb, smallb], writes=[Rb])
            sel = R[:, 16:32].rearrange("p (g e) -> p g e", e=4)
            prs = R[:, 32:56].rearrange("p (g e) -> p g e", e=6)
            pairs = [(0, 1), (0, 2), (0, 3), (1, 2), (1, 3), (2, 3)]
            for pi, (a, b) in enumerate(pairs):
                k.op(DVE, lambda e: e.tensor_tensor(out=prs[:, :, pi], in0=sel[:, :, a], in1=sel[:, :, b], op=ALU.add), reads=[Rb], writes=[Rb])
            k.op(DVE, lambda e: e.tensor_reduce(out=R[:, 96:97], in_=R[:, 32:56], axis=mybir.AxisListType.X, op=ALU.max), reads=[Rb], writes=[Rb])
            k.op(DVE, lambda e: e.tensor_scalar(out=R[:, 56:80], in0=R[:, 32:56], scalar1=R[:, 96:97], scalar2=None, op0=ALU.is_equal),
                 reads=[Rb], writes=[Rb])
            oh = R[:, 56:80].rearrange("p (g e) -> p g e", e=6)
            sl = R[:, 80:96].rearrange("p (g e) -> p g e", e=4)
            members = {0: (0, 1, 2), 1: (0, 3, 4), 2: (1, 3, 5), 3: (2, 4, 5)}
            for ei, (a, b, c) in members.items():
                k.op(DVE, lambda e: e.tensor_tensor(out=sl[:, :, ei], in0=oh[:, :, a], in1=oh[:, :, b], op=ALU.add), reads=[Rb], writes=[Rb])
                k.op(DVE, lambda e: e.tensor_tensor(out=sl[:, :, ei], in0=sl[:, :, ei], in1=oh[:, :, c], op=ALU.add), reads=[Rb], writes=[Rb])
            k.op(DVE, lambda e: e.tensor_tensor(out=R[:, 80:96], in0=R[:, 80:96], in1=R[:, 0:16], op=ALU.mult), reads=[Rb], writes=[Rb])
            k.op(DVE, lambda e: e.tensor_reduce(out=R[:, 97:98], in_=R[:, 80:96], axis=mybir.AxisListType.X, op=ALU.add), reads=[Rb], writes=[Rb])
            k.op(DVE, lambda e: e.reciprocal(out=R[:, 97:98], in_=R[:, 97:98]), reads=[Rb], writes=[Rb])
            k.op(DVE, lambda e: e.tensor_scalar(out=comb[:, m, :], in0=R[:, 80:96], scalar1=R[:, 97:98], scalar2=None, op0=ALU.mult),
                 reads=[Rb], writes=[combb[m]])
            if "CMB" in dbg:
                k.dma(SY, CMB[m * 128:(m + 1) * 128, :], comb[:, m, :], reads=[combb[m]], writes=[])

        def alloc_st(S):
            k.stF_r = Rot([(S.sb("stF%d" % i, [128, NT], BF16), bufs(5)) for i in range(2)])
            k.stT_r = Rot([(S.sb("stT%d" % i, [128, 512], BF16), Buf()) for i in range(3)])
        TG = [(0, 512), (512, 512), (1024, 512), (1536, 512), (2048, 256)]

        def proj_F(w, wb_, dst, r0, ncol=4, tgs=TG, scale=None):
            for jj in range(ncol):
                stF, stb = k.stF_r.next()
                for tgi_, (t0, tn) in enumerate(tgs):
                    pt, ptb = mmr.next()
                    for kc in range(16):
                        k.op(PE, lambda e: e.matmul(pt[:, 0:tn], w[:, kc, jj * 128:(jj + 1) * 128], k.BIG[:, kc, t0:t0 + tn],
                                                    start=(kc == 0), stop=(kc == 15)),
                             reads=[wb_, [BIGB[mm_][kc] for mm_ in range(t0 // 128, (t0 + tn) // 128)]], writes=[ptb])
                    if scale is None:
                        evac(stF[:, t0:t0 + tn], pt[:, 0:tn], [ptb], [stb[tgi_]])
                    else:
                        k.op(ACT, lambda e: e.activation(out=stF[:, t0:t0 + tn], in_=pt[:, 0:tn], func=AF.Copy, scale=scale), reads=[ptb], writes=[stb[tgi_]])
                tend = tgs[-1][0] + tgs[-1][1]
                k.dma(SY, dst[r0 + jj * 128:r0 + (jj + 1) * 128, 0:tend], stF[:, 0:tend], reads=[stb], writes=[])
                yield

        def proj_T(w, wb_, dst, c0, tiles):
            for m in tiles:
                pt, ptb = mmr.next()
                for kc in range(16):
                    k.op(PE, lambda e: e.matmul(pt[:, :], k.BIG[:, kc, m * 128:(m + 1) * 128], w[:, kc, :], start=(kc == 0), stop=(kc == 15)),
                         reads=[wb_, BIGB[m][kc]], writes=[ptb])
                stT, stb = k.stT_r.next()
                evac(stT[:, :], pt[:, :], [ptb], [stb])
                k.dma(SY, dst[m * 128:(m + 1) * 128, c0:c0 + 512], stT[:, :], reads=[stb], writes=[])
                if m % 4 == 3:
                    yield

        MAIN = [("F", AU_T, 0), ("F", AU_T, 512), ("T", AV, 0), ("T", AV, 512), ("F", BQ_T, 0), ("F", BQ_T, 512),
                ("F", BK_T, 0), ("F", BK_T, 512), ("T", BV, 0), ("T", BV, 512), ("F", CQ_T, 0), ("F", CK_T, 0),
                ("T", CV, 0), ("T", CV, 512), ("T", CO, 0), ("T", CO, 512)]

        def phase_inproj(l, S, last=False):
            tiles = list(range(NTILE))
            lat_only = {id(AU_T), id(BQ_T), id(CQ_T), id(AV), id(CO)} if last else set()
            bmg = S.sb("bmg", [128, 16], F32)
            wcg = S.sb("wcg", [128, 16, 16], BF16)
            cgs = S.sb("cgs", [128, NTILE, 16], F32)
            tmpb = Buf()
            k.dma(SY, bmg[:], bmg_in[l].partition_broadcast(128), writes=[tmpb])
            k.dma(GQ, wcg[:].rearrange("p a b -> p (a b)"), wcg_in[l], writes=[tmpb])

            def ld(i, slot):
                src = win_in[l, i] if i < 16 else wgate_in[l, i - 16]
                k.dma(GQ, slot[0][:].rearrange("p a b -> p (a b)"), src, writes=[slot[1]])
            st = WStream(k.wslot, ld, 28)
            for i in range(28):
                if i == 4:
                    yield "AUAV"
                w, wb_ = st.get(i)
                if i < 16:
                    mode, dst, off = MAIN[i]
                    lo_ = id(dst) in lat_only
                    if mode == "F":
                        yield from proj_F(w, wb_, dst, off, tgs=(TG[:4] if lo_ else TG), scale=(NA_SCALE if dst is BQ_T else None))
                    else:
                        yield from proj_T(w, wb_, dst, off, (list(range(16)) if lo_ else tiles))
                else:
                    yield from proj_F(w, wb_, GATE_T, (i - 16) * 512, tgs=(TG[:4] if last else TG))
            for m in tiles:
                pt, ptb = auxr.next()
                for kc in range(16):
                    k.op(PE, lambda e: e.matmul(pt[:, 0:16], k.BIG[:, kc, m * 128:(m + 1) * 128], wcg[:, kc, :], start=(kc == 0), stop=(kc == 15)),
                         reads=[tmpb, BIGB[m][kc]], writes=[ptb])
                k.op(DVE, lambda e: e.tensor_tensor(out=cgs[:, m, :], in0=pt[:, 0:16], in1=bmg[:], op=ALU.add), reads=[ptb, tmpb], writes=[tmpb])
            k.dma(SY, CG.rearrange("(m p) g -> p m g", p=128), cgs[:], reads=[tmpb], writes=[])

        def fence_bufs():
            b = Buf()
            for s, c in SY.sems:
                if c > 0:
                    b.w[s] = c
            for s, c in GQ.sems:
                if c > 0:
                    b.w[s] = c
            return b

        def gelu_(dst, src, tmp, reads, wr):
            k.op(ACT, lambda e: e.activation(out=tmp, in_=src, func=AF.Square), reads=reads, writes=[wr])
            k.op(DVE, lambda e: e.scalar_tensor_tensor(out=tmp, in0=tmp, scalar=1.0 / 0.044715, in1=src, op0=ALU.add, op1=ALU.mult), reads=[wr] + reads, writes=[wr])
            k.op(ACT, lambda e: e.activation(out=tmp, in_=tmp, func=AF.Sigmoid, scale=GELU_C * 0.044715), reads=[wr], writes=[wr])
            k.op(DVE, lambda e: e.tensor_tensor(out=dst, in0=tmp, in1=src, op=ALU.mult), reads=[wr] + reads, writes=[wr])

        def gelu2(items):
            for (dst, src, tmp, reads, wr) in items:
                k.op(ACT, lambda e: e.activation(out=tmp, in_=src, func=AF.Square), reads=reads, writes=[wr])
            for (dst, src, tmp, reads, wr) in items:
                k.op(DVE, lambda e: e.scalar_tensor_tensor(out=tmp, in0=tmp, scalar=1.0 / 0.044715, in1=src, op0=ALU.add, op1=ALU.mult), reads=[wr] + reads, writes=[wr])
            for (dst, src, tmp, reads, wr) in items:
                k.op(ACT, lambda e: e.activation(out=tmp, in_=tmp, func=AF.Sigmoid, scale=GELU_C * 0.044715), reads=[wr], writes=[wr])
            for (dst, src, tmp, reads, wr) in items:
                k.op(DVE, lambda e: e.tensor_tensor(out=dst, in0=tmp, in1=src, op=ALU.mult), reads=[wr] + reads, writes=[wr])

        def phase_sgu(l, fb, tiles, sb, Q=None):
            Q = Q or SY
            if True:
                gs = sb("gs", [128, 1024], F32)
                wspf = sb("wspf", [128, 8, 128], F32)
                wspb = sb("wspb", [128, 8, 128], BF16)
                bspf = sb("bspf", [1, 1024], F32)
                bspb = sb("bspb", [1, 1024], BF16)
                cb = Buf()
                k.dma(Q, gs[:], gsgu_in[l].partition_broadcast(128), writes=[cb])
                k.dma(Q, wspf[:].rearrange("p a b -> p (a b)"), wsp_in[l], writes=[cb])
                k.dma(Q, bspf[:], bsp_in[l:l + 1, :], writes=[cb])
                k.op(ACT, lambda e: e.activation(out=wspb[:], in_=wspf[:], func=AF.Copy), reads=[cb], writes=[cb])
                k.op(ACT, lambda e: e.activation(out=bspb[:], in_=bspf[:], func=AF.Copy), reads=[cb], writes=[cb])
                avr = Rot([(sb("av%d" % i, [128, 1024], BF16), Buf()) for i in range(2)])
                aur = Rot([(sb("au%d" % i, [128, 8, 128], BF16), Buf()) for i in range(2)])
                t1r = Rot([(sb("t1%d" % i, [128, 1024], F32), Buf()) for i in range(2)])
                gvr = Rot([(sb("gv%d" % i, [128, 1024], F32), Buf()) for i in range(2)])
                gur = Rot([(sb("gu%d" % i, [128, 1024], F32), Buf()) for i in range(2)])
                vnr = Rot([(sb("vn%d" % i, [128, 1024], BF16), Buf()) for i in range(2)])
                yar = Rot([(sb("ya%d" % i, [128, 8, 128], BF16), Buf()) for i in range(2)])
                AUv = AU_T.rearrange("(g p) t -> p g t", p=128)
                YAv = YA_T.rearrange("(g p) t -> p g t", p=128)
                sld = {}
                spend = []

                def sgu_mm(c, vn, vnb, gu, t2b):
                    ya, yab = yar.next()
                    for half in range(2):
                        pt, ptb = auxr.next()
                        for gi in range(4):
                            g = half * 4 + gi
                            k.op(PE, lambda e: e.matmul(pt[:, gi * 128:(gi + 1) * 128], vn[:, g * 128:(g + 1) * 128], wspb[:, g, :], start=True, stop=False),
                                 reads=[vnb, cb], writes=[ptb])
                            k.op(PE, lambda e: e.matmul(pt[:, gi * 128:(gi + 1) * 128], onesB[0:1, :], bspb[0:1, g * 128:(g + 1) * 128], start=False, stop=True),
                                 reads=[cbb, cb], writes=[ptb])
                        k.op(DVE, lambda e: e.tensor_tensor(out=ya[:, half * 4:half * 4 + 4, :].rearrange("p a b -> p (a b)"), in0=pt[:, :],
                                                            in1=gu[:, half * 512:(half + 1) * 512], op=ALU.mult),
                             reads=[ptb, t2b], writes=[yab])
                    k.dma(Q, YAv[:, :, c * 128:(c + 1) * 128], ya[:], reads=[yab], writes=[])

                def issue_s(c_):
                    av_, avb_ = avr.next()
                    au_, aub_ = aur.next()
                    k.dma(Q, av_[:], AV[c_ * 128:(c_ + 1) * 128, :], reads=[fb], writes=[avb_])
                    k.dma(Q, au_[:], AUv[:, :, c_ * 128:(c_ + 1) * 128], reads=[fb], writes=[aub_])
                    sld[c_] = (av_, avb_, au_, aub_)
                issue_s(tiles[0])
                for ci_, c in enumerate(tiles):
                    if ci_ + 1 < len(tiles):
                        issue_s(tiles[ci_ + 1])
                    av, avb, au, aub = sld.pop(c)
                    t1, t1b = t1r.next()
                    gv, gvb = gvr.next()
                    gu, gub = gur.next()
                    t2, t2b = t1r.next()
                    gelu2([(gv[:], av[:], t1[:], [avb], t1b), (gu[:], au[:].rearrange("p a b -> p (a b)"), t2[:], [aub], t2b)])
                    ss, ssb = sc_r.next()
                    k.op(DVE, lambda e: e.memset(ss[:, 0:1], 0.0), writes=[ssb])
                    k.op(ACT, lambda e: e.activation(out=t1[:], in_=gv[:], func=AF.Square, accum_out=ss[:, 0:1]), reads=[t1b], writes=[t1b, ssb])
                    rstd_of(ss[:, 0:1], ssb, 1024)
                    vn, vnb = vnr.next()
                    k.op(DVE, lambda e: e.scalar_tensor_tensor(out=vn[:], in0=gv[:], scalar=ss[:, 0:1], in1=gs[:], op0=ALU.mult, op1=ALU.mult),
                         reads=[t1b, ssb, cb], writes=[vnb])
                    if spend:
                        sgu_mm(*spend.pop())
                    spend.append((c, vn, vnb, gu, t2b))
                    yield
                sgu_mm(*spend.pop())
                yield

        def phase_na(l, fb, with_ctx_q, sb):
            hbufs = Rot([dict(q=sb("naq%d" % i, [128, NT], BF16), k=sb("nak%d" % i, [128, NT], BF16), v=sb("nav%d" % i, [128, NTILE, 128], BF16),
                              b=Buf()) for i in range(2)])
            Ht = sb("nat", [128, 25, 128], BF16)
            htb = Buf()
            yr_ = Rot([(sb("nay%d" % i, [128, NT], BF16), Buf()) for i in range(2)])
            tmr = Rot([(sb("natm%d" % i, [128, 5 * 128], F32), Buf()) for i in range(2)])
            ptr_ = Rot([(sb("napt%d" % i, [128, 7 * 128], BF16), Buf()) for i in range(3)])
            rdr = Rot([(sb("nard%d" % i, [128, 128], F32), Buf()) for i in range(2)])
            BVv = BV.rearrange("(m p) f -> p m f", p=128)
            nq = NTILE if with_ctx_q else 16
            def ld_head(h_):
                H_ = hbufs.next()
                k.dma(SY, H_["q"][:], BQ_T[h_ * 128:(h_ + 1) * 128, :], reads=[fb], writes=[H_["b"]])
                k.dma(SY, H_["k"][:], BK_T[h_ * 128:(h_ + 1) * 128, :], reads=[fb], writes=[H_["b"]])
                k.dma(SY, H_["v"][:], BVv[:, :, h_ * 128:(h_ + 1) * 128], reads=[fb], writes=[H_["b"]])
                return H_
            nxt_head = ld_head(0)
            for h in range(8):
                HH = nxt_head
                if h + 1 < 8:
                    nxt_head = ld_head(h + 1)
                Hq, Hk, Hv, hb = HH["q"], HH["k"], HH["v"], HH["b"]
                k.dma(GQ, Ht[:].rearrange("p a b -> p (a b)"), natab_in[l, h], writes=[htb])
                Hy, Hyb = yr_.next()

                def stageA(p):
                    if p < 16:
                        wt = na_tiles(p)
                        var = na_var(p)
                    else:
                        wt = []
                        var = 0
                    tiles = wt + [16, 17]
                    nw = len(wt)
                    pa, pab = mm[0]
                    pb_, pbb = mm[1]

                    def sreg(i):
                        return (pa, pab, i) if i < 4 else (pb_, pbb, i - 4)
                    for i, t in enumerate(tiles):
                        pt, ptb, ii = sreg(i)
                        isw = i < nw
                        k.op(PE, lambda e: e.matmul(pt[:, ii * 128:(ii + 1) * 128], Hk[:, t * 128:(t + 1) * 128], Hq[:, p * 128:(p + 1) * 128],
                                                    start=True, stop=(not isw)), reads=[hb], writes=[ptb])
                        if isw:
                            k.op(PE, lambda e: e.matmul(pt[:, ii * 128:(ii + 1) * 128], identB[:, :], Ht[:, var * 5 + i, :], start=False, stop=True),
                                 reads=[cbb, htb], writes=[ptb])
                    pT, pTb = ptr_.next()
                    nt_ = len(tiles)
                    na_ = min(nt_, 4)
                    k.op(ACT, lambda e: e.activation(out=pT[:, 0:na_ * 128], in_=pa[:, 0:na_ * 128], func=AF.Exp), reads=[pab], writes=[pTb])
                    if nt_ > 4:
                        k.op(ACT, lambda e: e.activation(out=pT[:, 512:nt_ * 128], in_=pb_[:, 0:(nt_ - 4) * 128], func=AF.Exp), reads=[pbb], writes=[pTb])
                    return (p, tiles, pT, pTb)

                def stageB(st):
                    p, tiles, pT, pTb = st
                    po, pob = aux[0]
                    nt_ = len(tiles)
                    for i, t in enumerate(tiles):
                        k.op(PE, lambda e: e.matmul(po[:, 0:128], Hv[:, t, :], pT[:, i * 128:(i + 1) * 128], start=(i == 0), stop=(i == nt_ - 1)),
                             reads=[hb, pTb], writes=[pob])
                    for i, t in enumerate(tiles):
                        k.op(PE, lambda e: e.matmul(po[:, 128:256], onesB[:, :], pT[:, i * 128:(i + 1) * 128], start=(i == 0), stop=(i == nt_ - 1)),
                             reads=[cbb, pTb], writes=[pob])
                    rd, rdb = rdr.next()
                    k.op(DVE, lambda e: e.reciprocal(out=rd[:], in_=po[:, 128:256]), reads=[pob], writes=[rdb])
                    k.op(DVE, lambda e: e.tensor_tensor(out=Hy[:, p * 128:(p + 1) * 128], in0=po[:, 0:128], in1=rd[:], op=ALU.mult),
                         reads=[pob, rdb], writes=[Hyb])
                pend = None
                for p in range(nq):
                    cur = stageA(p)
                    if pend is not None:
                        stageB(pend)
                        yield
                    pend = cur
                stageB(pend)
                k.dma(SY, YB_T[h * 128:(h + 1) * 128, 0:nq * 128], Hy[:, 0:nq * 128], reads=[Hyb], writes=[])
                yield

        def phase_rope(fb):
            with contextlib.ExitStack() as les:
                def sb(name, shape, dt):
                    return les.enter_context(nc.sbuf_tensor(un(name), shape, dt))
                rope = sb("rope", [128, 4, SEQ], F32)
                gb = Buf()
                k.dma(SY, rope[:].rearrange("p a b -> p (a b)"), rope_in[:, :], writes=[gb])
                rawr = Rot([(sb("rraw%d" % i, [128, NT], BF16), Buf()) for i in range(2)])
                dstr = Rot([(sb("rdst%d" % i, [128, NT], BF16), Buf()) for i in range(2)])
                rt1 = Rot([(sb("mrt%d" % i, [128, 512], F32), Buf()) for i in range(2)])
                rt2 = Rot([(sb("mru%d" % i, [128, 512], F32), Buf()) for i in range(2)])
                rjobs = [(h, T_, ti, isq) for h in range(4) for (T_, ti, isq) in ((CQ_T, 0, True), (CK_T, 2, False))]
                rpend = {}

                def issue_rope(ji):
                    h_, T__, _, _ = rjobs[ji]
                    raw_, rb_ = rawr.next()
                    k.dma(SY, raw_[:], T__[h_ * 128:(h_ + 1) * 128, :], reads=[fb], writes=[rb_])
                    rpend[ji] = (raw_, rb_)
                issue_rope(0)
                for ji, (h, T_, ti, isq) in enumerate(rjobs):
                    if True:
                        if ji + 1 < len(rjobs):
                            issue_rope(ji + 1)
                        raw, rb = rpend.pop(ji)
                        dst, db = dstr.next()
                        for tg in range(4):
                            sl = slice(tg * 512, (tg + 1) * 512)
                            pt, ptb = mmr.next()
                            k.op(PE, lambda e: e.matmul(pt[:, :], permB[:, :], raw[:, sl], start=True, stop=True), reads=[rb, cbb], writes=[ptb])
                            a, ab = rt1.next()
                            b, bb = rt2.next()
                            k.op(DVE, lambda e: e.tensor_tensor(out=a[:], in0=pt[:, :], in1=rope[:, ti + 1, sl], op=ALU.mult), reads=[ptb, gb], writes=[ab])
                            k.op(DVE, lambda e: e.tensor_tensor(out=b[:], in0=raw[:, sl], in1=rope[:, ti, sl], op=ALU.mult), reads=[rb, gb], writes=[bb])
                            k.op(DVE, lambda e: e.tensor_tensor(out=dst[:, sl], in0=a[:], in1=b[:], op=ALU.add), reads=[ab, bb], writes=[db])
                        if isq:
                            k.op(ACT, lambda e: e.activation(out=dst[:, SEQ:NT], in_=raw[:, SEQ:NT], func=AF.Copy, scale=NA_SCALE), reads=[rb], writes=[db])
                        else:
                            k.op(ACT, lambda e: e.activation(out=dst[:, SEQ:NT], in_=raw[:, SEQ:NT], func=AF.Copy), reads=[rb], writes=[db])
                        k.dma(SY, T_[h * 128:(h + 1) * 128, :], dst[:], reads=[db, rb], writes=[])

        def phase_mlstm(l, fb, ctx_out, sb):
            gmn = sb("mgmn", [128, 1024], F32)
            G = sb("mG", [128, NTILE, 16], F32)
            T1 = sb("mT1", [128, NTILE, 16], F32)
            LF = sb("mLF", [128, NTILE, 16], F32)
            CUM = sb("mCUM", [128, NTILE, 48], F32)
            A = sb("mA", [128, NTILE, 8], F32)
            KS = sb("mKS", [128, NTILE, 8], F32)
            DEC = sb("mDEC", [128, NTILE, 8], F32)
            AINV = sb("mAINV", [128, NTILE, 8], F32)
            KSD = sb("mKSD", [128, NTILE, 8], F32)
            gb = Buf()
            k.dma(SY, gmn[:], gmn_in[l].partition_broadcast(128), writes=[gb])
            k.dma(SY, G[:], CG.rearrange("(m p) g -> p m g", p=128), reads=[fb], writes=[gb])
            k.op(ACT, lambda e: e.activation(out=T1[:], in_=G[:], func=AF.Exp, scale=-1.0), reads=[gb], writes=[gb])
            k.op(DVE, lambda e: e.tensor_scalar(out=T1[:], in0=T1[:], scalar1=1.0, scalar2=None, op0=ALU.add), reads=[gb], writes=[gb])
            k.op(ACT, lambda e: e.activation(out=T1[:], in_=T1[:], func=AF.Ln), reads=[gb], writes=[gb])
            k.op(DVE, lambda e: e.tensor_scalar(out=LF[:], in0=T1[:], scalar1=-1.0, scalar2=None, op0=ALU.mult), reads=[gb], writes=[gb])
            for c in range(NTILE):
                pt, ptb = auxr.next()
                k.op(PE, lambda e: e.matmul(pt[:, 0:16], triF[0], LF[:, c, :], start=True, stop=True), reads=[cstb, gb], writes=[ptb])
                k.op(PE, lambda e: e.matmul(pt[:, 16:32], triF[1], LF[:, c, :], start=True, stop=True), reads=[cstb, gb], writes=[ptb])
                k.op(PE, lambda e: e.matmul(pt[:, 32:48], onesF, LF[:, c, :], start=True, stop=True), reads=[cstb, gb], writes=[ptb])
                k.op(DVE, lambda e: e.tensor_copy(out=CUM[:, c, :], in_=pt[:, 0:48]), reads=[ptb], writes=[gb])
            for d_, (bo, io, to) in enumerate([(4, 0, 36), (28, 8, 44)]):
                k.op(ACT, lambda e: e.activation(out=A[:, :, d_ * 4:d_ * 4 + 4], in_=CUM[:, :, bo:bo + 4], func=AF.Exp), reads=[gb], writes=[gb])
                k.op(ACT, lambda e: e.activation(out=AINV[:, :, d_ * 4:d_ * 4 + 4], in_=CUM[:, :, bo:bo + 4], func=AF.Exp, scale=-1.0), reads=[gb], writes=[gb])
                k.op(DVE, lambda e: e.tensor_tensor(out=KS[:, :, d_ * 4:d_ * 4 + 4], in0=G[:, :, io:io + 4], in1=CUM[:, :, bo:bo + 4], op=ALU.subtract),
                     reads=[gb], writes=[gb])
                k.op(ACT, lambda e: e.activation(out=KS[:, :, d_ * 4:d_ * 4 + 4], in_=KS[:, :, d_ * 4:d_ * 4 + 4], func=AF.Exp), reads=[gb], writes=[gb])
                k.op(ACT, lambda e: e.activation(out=DEC[:, :, d_ * 4:d_ * 4 + 4], in_=CUM[:, :, to:to + 4], func=AF.Exp), reads=[gb], writes=[gb])
            k.op(DVE, lambda e: e.tensor_tensor(out=KSD[:], in0=KS[:], in1=DEC[:], op=ALU.mult), reads=[gb], writes=[gb])
            yield
            Hs = sb("mH", [128, NTILE, 256], F32)
            sg32 = sb("msg32", [128, NTILE, 256], F32)
            sgb32 = Buf()
            ss18 = sb("mss18", [128, NTILE], F32)
            ss18b = Buf()
            junk = sb("mjunk", [128, 256], F32)
            junkb = Buf()
            Hb = bufs(NTILE)
            mbufs = Rot([dict(q=sb("mqT%d" % i, [128, NT], BF16), k=sb("mkT%d" % i, [128, NT], BF16), v=sb("mVp%d" % i, [128, NTILE, 257], BF16),
                              c=sb("mco%d" % i, [128, NTILE, 256], BF16), b=Buf()) for i in range(2)])
            spr = Rot([(sb("msp%d" % i, [128, 128], BF16), Buf()) for i in range(4)])
            ktr = Rot([(sb("mkt%d" % i, [128, 128], BF16), Buf()) for i in range(4)])
            tcr = Rot([(sb("mtc%d" % i, [128, 257], F32), Buf()) for i in range(2)])
            Cst = [(sb("mCs%d" % i, [128, 257], F32), Buf()) for i in range(2)]
            Cbf = [(sb("mCb%d" % i, [128, 257], BF16), Buf()) for i in range(2)]
            yr = Rot([(sb("my%d" % i, [128, 256], F32), Buf()) for i in range(2)])
            sgr_ = Rot([(sb("mys%d" % i, [128, 256], F32), Buf()) for i in range(2)])
            ybr = Rot([(sb("myb%d" % i, [128, 256], BF16), Buf()) for i in range(2)])
            ytr = Rot([(sb("myt%d" % i, [128, 2, 128], BF16), Buf()) for i in range(2)])
            CVv = CV.rearrange("(m p) f -> p m f", p=128)
            COv = CO.rearrange("(m p) f -> p m f", p=128)
            YCv = YC_T.rearrange("(g p) t -> p g t", p=128)
            orders = [[16, 17] + list(range(16)), [17, 16] + list(range(15, -1, -1))]
            def ld_mh(h_):
                M_ = mbufs.next()
                k.dma(SY, M_["q"][:], CQ_T[h_ * 128:(h_ + 1) * 128, :], reads=[fb], writes=[M_["b"]])
                k.dma(SY, M_["k"][:], CK_T[h_ * 128:(h_ + 1) * 128, :], reads=[fb], writes=[M_["b"]])
                k.dma(SY, M_["v"][:, :, 0:256], CVv[:, :, h_ * 256:(h_ + 1) * 256], reads=[fb], writes=[M_["b"]])
                k.dma(SY, M_["c"][:], COv[:, :, h_ * 256:(h_ + 1) * 256], reads=[fb], writes=[M_["b"]])
                k.op(DVE, lambda e: e.memset(M_["v"][:, :, 256:257], 1.0), writes=[M_["b"]])
                return M_
            nxt_mh = ld_mh(0)
            for h in range(4):
                MM = nxt_mh
                if h + 1 < 4:
                    nxt_mh = ld_mh(h + 1)
                qT, kT, Vp, coh, hb = MM["q"], MM["k"], MM["v"], MM["c"], MM["b"]
                for d_ in range(2):
                    k.op(DVE, lambda e: e.memset(Cst[d_][0][:], 0.0), writes=[Cst[d_][1]])
                    k.op(DVE, lambda e: e.memset(Cbf[d_][0][:], 0.0), writes=[Cbf[d_][1]])
                written = [False] * NTILE
                for step in range(NTILE):
                    recs = []
                    for d_ in range(2):
                        c = orders[d_][step]
                        r_ = dict(d=d_, c=c, dh=d_ * 4 + h, cs=slice(c * 128, (c + 1) * 128), want=((c < 16) or ctx_out), upd=(step < NTILE - 1))
                        recs.append(r_)
                        cs = r_["cs"]
                        if r_["want"]:
                            pSt, pSb = aux[1]
                            pS = pSt[:, d_ * 128:(d_ + 1) * 128]
                            k.op(PE, lambda e: e.matmul(pS, kT[:, cs], qT[:, cs], start=True, stop=True), reads=[hb], writes=[pSb])
                            r_["pS"], r_["pSb"] = pS, pSb
                        if r_["upd"]:
                            pT_, pTb_ = tb[d_]
                            k.op(PE, lambda e: e.transpose(pT_[:, 0:128], kT[:, cs], identB[:]), reads=[hb, cbb], writes=[pTb_])
                            r_["pT"], r_["pTb"] = pT_, pTb_
                    for r_ in recs:
                        d_, c, dh = r_["d"], r_["c"], r_["dh"]
                        if r_["want"]:
                            pS, pSb = r_["pS"], r_["pSb"]
                            sp, spb = spr.next()
                            k.op(DVE, lambda e: e.scalar_tensor_tensor(out=sp[:], in0=pS, scalar=KS[:, c, dh:dh + 1], in1=triF[d_],
                                                                       op0=ALU.mult, op1=ALU.mult), reads=[pSb, gb, cstb], writes=[spb])
                            r_["sp"], r_["spb"] = sp, spb
                        if r_["upd"]:
                            pT_, pTb_ = r_["pT"], r_["pTb"]
                            kt, ktb = ktr.next()
                            k.op(ACT, lambda e: e.activation(out=kt[:], in_=pT_[:, 0:128], func=AF.Copy, scale=KSD[:, c, dh:dh + 1]), reads=[pTb_, gb], writes=[ktb])
                            r_["kt"], r_["ktb"] = kt, ktb
                    for r_ in recs:
                        d_, c = r_["d"], r_["c"]
                        if r_["upd"]:
                            kt, ktb = r_["kt"], r_["ktb"]
                            pC, pCb = mm[2 + d_]
                            k.op(PE, lambda e: e.matmul(pC[:, 0:257], kt[:], Vp[:, c, :], start=True, stop=True), reads=[ktb, hb], writes=[pCb])
                            r_["pC"], r_["pCb"] = pC, pCb
                    for r_ in recs:
                        d_, c, dh = r_["d"], r_["c"], r_["dh"]
                        if r_["upd"]:
                            pC, pCb = r_["pC"], r_["pCb"]
                            k.op(DVE, lambda e: e.scalar_tensor_tensor(out=Cst[d_][0][:], in0=Cst[d_][0][:], scalar=DEC[:, c, dh:dh + 1], in1=pC[:, 0:257],
                                                                       op0=ALU.mult, op1=ALU.add), reads=[pCb, Cst[d_][1], gb], writes=[Cst[d_][1]])
                    for r_ in recs:
                        d_, c, cs = r_["d"], r_["c"], r_["cs"]
                        if r_["want"]:
                            sp, spb = r_["sp"], r_["spb"]
                            pN, pNb = mm[2 + d_]
                            k.op(PE, lambda e: e.matmul(pN[:, 0:257], sp[:], Vp[:, c, :], start=True, stop=False), reads=[spb, hb], writes=[pNb])
                            k.op(PE, lambda e: e.matmul(pN[:, 0:257], qT[:, cs], Cbf[d_][0][:], start=False, stop=True), reads=[hb, Cbf[d_][1]], writes=[pNb])
                            r_["pN"], r_["pNb"] = pN, pNb
                    for r_ in recs:
                        d_ = r_["d"]
                        if r_["upd"]:
                            k.op(ACT, lambda e: e.activation(out=Cbf[d_][0][:], in_=Cst[d_][0][:], func=AF.Copy), reads=[Cst[d_][1]], writes=[Cbf[d_][1]])
                    for r_ in recs:
                        d_, c, dh = r_["d"], r_["c"], r_["dh"]
                        if r_["want"]:
                            pN, pNb = r_["pN"], r_["pNb"]
                            s3, s3b = sc_r.next()
                            k.op(DVE, lambda e: e.tensor_tensor(out=s3[:, 0:1], in0=pN[:, 256:257], in1=AINV[:, c, dh:dh + 1], op=ALU.max), reads=[pNb, gb], writes=[s3b])
                            k.op(DVE, lambda e: e.scalar_tensor_tensor(out=s3[:, 2:3], in0=pN[:, 256:257], scalar=-1.0, in1=s3[:, 0:1], op0=ALU.mult, op1=ALU.max), reads=[pNb, s3b], writes=[s3b])
                            k.op(DVE, lambda e: e.reciprocal(out=s3[:, 1:2], in_=s3[:, 2:3]), reads=[s3b], writes=[s3b])
                            hdst = Hs[:, c, :]
                            if not written[c]:
                                k.op(ACT, lambda e: e.activation(out=hdst, in_=pN[:, 0:256], func=AF.Copy, scale=s3[:, 1:2]), reads=[pNb, s3b], writes=[Hb[c]])
                                written[c] = True
                            else:
                                k.op(DVE, lambda e: e.scalar_tensor_tensor(out=hdst, in0=pN[:, 0:256], scalar=s3[:, 1:2], in1=hdst, op0=ALU.mult, op1=ALU.add),
                                     reads=[pNb, s3b], writes=[Hb[c]])
                    yield
                ntl = NTILE if ctx_out else 16
                k.op(ACT, lambda e: e.activation(out=sg32[:, 0:ntl, :], in_=coh[:, 0:ntl, :], func=AF.Exp, scale=-1.0), reads=[hb], writes=[sgb32])
                k.op(ACT, lambda e: e.activation(out=sg32[:, 0:ntl, :], in_=sg32[:, 0:ntl, :], func=AF.Ln, bias=epsT[:, 1:2]), reads=[sgb32, epsb], writes=[sgb32])
                k.op(ACT, lambda e: e.activation(out=sg32[:, 0:ntl, :], in_=sg32[:, 0:ntl, :], func=AF.Exp, scale=-1.0), reads=[sgb32], writes=[sgb32])
                k.op(DVE, lambda e: e.memset(ss18[:], 0.0), writes=[ss18b])
                for c in range(ntl):
                    k.op(ACT, lambda e: e.activation(out=junk[:], in_=Hs[:, c, :], func=AF.Square, accum_out=ss18[:, c:c + 1]), reads=[Hb[c]], writes=[junkb, ss18b])
                rstd_of(ss18[:, 0:ntl], ss18b, 256)
                yield
                opend = []

                def out_back(c, ybf, ybfb, h=h):
                    pT_, pTb_ = tbr.next()
                    for g in range(2):
                        k.op(PE, lambda e: e.transpose(pT_[:, g * 128:(g + 1) * 128], ybf[:, g * 128:(g + 1) * 128], identB[:]), reads=[ybfb, cbb], writes=[pTb_])
                    yt, ytb = ytr.next()
                    evac(yt[:].rearrange("p a b -> p (a b)"), pT_[:, 0:256], [pTb_], [ytb])
                    k.dma(SY, YCv[:, 2 * h:2 * h + 2, c * 128:(c + 1) * 128], yt[:], reads=[ytb], writes=[])
                for c in range(ntl):
                    y, yb_ = yr.next()
                    k.op(DVE, lambda e: e.scalar_tensor_tensor(out=y[:], in0=Hs[:, c, :], scalar=ss18[:, c:c + 1], in1=gmn[:, h * 256:(h + 1) * 256], op0=ALU.mult, op1=ALU.mult),
                         reads=[Hb[c], ss18b, gb], writes=[yb_])
                    ybf, ybfb = ybr.next()
                    k.op(DVE, lambda e: e.tensor_tensor(out=ybf[:], in0=y[:], in1=sg32[:, c, :], op=ALU.mult), reads=[yb_, sgb32], writes=[ybfb])
                    if opend:
                        out_back(*opend.pop())
                    opend.append((c, ybf, ybfb))
                    if c % 2 == 1:
                        yield
                out_back(*opend.pop())

        def phase_merge(l, fb, tgs):
            with contextlib.ExitStack() as les:
                def sb(name, shape, dt):
                    return les.enter_context(nc.sbuf_tensor(un(name), shape, dt))
                tend = tgs[-1][0] + tgs[-1][1]
                Y = [sb("mgY%d" % i, [128, 8, NT], BF16) for i in range(3)]
                yb_ = Buf()
                for i, src in enumerate((YA_T, YB_T, YC_T)):
                    k.dma(SY, Y[i][:, :, 0:tend], src.rearrange("(g p) t -> p g t", p=128)[:, :, 0:tend], reads=[fb], writes=[yb_])
                wbs = [(sb("mgw%d" % i, [128, 3, 8, 128], BF16), Buf()) for i in range(2)]
                gts = Rot([(sb("mgg%d" % i, [128, 3, NT], BF16), Buf()) for i in range(2)])
                tr_ = Rot([(sb("mgt%d" % i, [128, 512], F32), Buf()) for i in range(4)])
                mgr = Rot([(sb("mgm%d" % i, [128, NT], BF16), Buf()) for i in range(2)])
                GTv = GATE_T.rearrange("(i f p) t -> f p i t", i=3, p=128)

                def ld(f, slot):
                    k.dma(GQ, slot[0][:].rearrange("p a b c -> p (a b c)"), wbr_in[l, f], writes=[slot[1]])
                st = WStream(wbs, ld, 16)
                def prep_gate(f_):
                    gt_, gtb_ = gts.next()
                    k.dma(SY, gt_[:, :, 0:tend], GTv[f_][:, :, 0:tend], reads=[fb], writes=[gtb_])
                    k.op(ACT, lambda e: e.activation(out=gt_[:, :, 0:tend], in_=gt_[:, :, 0:tend], func=AF.Sigmoid), reads=[gtb_], writes=[gtb_])
                    return gt_, gtb_
                nxt_gate = prep_gate(0)
                for f in range(16):
                    w, wb_ = st.get(f)
                    gt, gtb = nxt_gate
                    if f + 1 < 16:
                        nxt_gate = prep_gate(f + 1)
                    sg, sgb = gt, gtb
                    mg, mgb = mgr.next()
                    for (t0, tn) in tgs:
                        ts_ = []
                        for i in range(3):
                            pt, ptb = mmr.next()
                            for kc in range(8):
                                k.op(PE, lambda e: e.matmul(pt[:, 0:tn], w[:, i, kc, :], Y[i][:, kc, t0:t0 + tn], start=(kc == 0), stop=(kc == 7)),
                                     reads=[wb_, yb_], writes=[ptb])
                            t, tb_ = tr_.next()
                            k.op(DVE, lambda e: e.tensor_tensor(out=t[:, 0:tn], in0=pt[:, 0:tn], in1=sg[:, i, t0:t0 + tn], op=ALU.mult), reads=[ptb, sgb], writes=[tb_])
                            ts_.append((t, tb_))
                        k.op(DVE, lambda e: e.tensor_tensor(out=ts_[0][0][:, 0:tn], in0=ts_[0][0][:, 0:tn], in1=ts_[1][0][:, 0:tn], op=ALU.add),
                             reads=[ts_[0][1], ts_[1][1]], writes=[ts_[0][1]])
                        k.op(DVE, lambda e: e.tensor_tensor(out=mg[:, t0:t0 + tn], in0=ts_[0][0][:, 0:tn], in1=ts_[2][0][:, 0:tn], op=ALU.add),
                             reads=[ts_[0][1], ts_[2][1]], writes=[mgb])
                    k.dma(SY, MG_T[f * 128:(f + 1) * 128, 0:tend], mg[:, 0:tend], reads=[mgb], writes=[])

        def phase_out(l, fb, tiles):
            with contextlib.ExitStack() as les:
                def sb(name, shape, dt):
                    return les.enter_context(nc.sbuf_tensor(un(name), shape, dt))
                tend = (tiles[-1] + 1) * 128
                k.dma(SY, k.BIG[:, :, 0:tend], MG_T.rearrange("(c p) t -> p c t", p=128)[:, :, 0:tend], reads=[fb], writes=BIGB)
                gt1 = sb("ogt", [128, 2, D], F32)
                gb = Buf()
                for v in range(2):
                    k.dma(SY, gt1[:, v, :], MODROW[l, v, 32 * 128:48 * 128].partition_broadcast(128), reads=[modb], writes=[gb])
                xr = Rot([(sb("ox%d" % i, [128, 512], F32), Buf()) for i in range(3)])
                tr_ = Rot([(sb("ot%d" % i, [128, 512], F32), Buf()) for i in range(3)])

                def ld(n, slot):
                    k.dma(GQ, slot[0][:].rearrange("p a b -> p (a b)"), wout_in[l, n], writes=[slot[1]])
                st = WStream(k.wslot, ld, 4)
                its = [(n_, m_) for n_ in range(4) for m_ in tiles]
                xld = {}
                nld = [0]

                def ensure_x(upto):
                    while nld[0] <= min(upto, len(its) - 1):
                        n_, m_ = its[nld[0]]
                        xs_, xsb_ = xr.next()
                        src_, srcb_ = x_src(l, 1, m_)
                        k.dma(SY, xs_[:], src_[:, n_ * 512:(n_ + 1) * 512], reads=(srcb_[n_] if srcb_ else []), writes=[xsb_])
                        xld[nld[0]] = (xs_, xsb_)
                        nld[0] += 1
                for n in range(4):
                    w, wb_ = st.get(n)
                    for mi_, m in enumerate(tiles):
                        v = 0 if m < 16 else 1
                        it_ = n * len(tiles) + mi_
                        ensure_x(it_ + 2)
                        xs, xsb = xld.pop(it_)
                        pt, ptb = mmr.next()
                        for kc in range(16):
                            k.op(PE, lambda e: e.matmul(pt[:, :], k.BIG[:, kc, m * 128:(m + 1) * 128], w[:, kc, :], start=(kc == 0), stop=(kc == 15)),
                                 reads=[wb_, BIGB[m][kc]], writes=[ptb])
                        t, tb_ = tr_.next()
                        k.op(DVE, lambda e: e.tensor_tensor(out=t[:], in0=pt[:, :], in1=gt1[:, v, n * 512:(n + 1) * 512], op=ALU.mult), reads=[ptb, gb], writes=[tb_])
                        k.op(DVE, lambda e: e.tensor_tensor(out=t[:], in0=t[:], in1=xs[:], op=ALU.add), reads=[tb_, xsb], writes=[tb_])
                        k.dma(SY, XL[m * 128:(m + 1) * 128, n * 512:(n + 1) * 512], t[:], reads=[tb_], writes=[XB[m][n]])

        def phase_moe(l, tiles, last, fb2):
            with contextlib.ExitStack() as les:
                def sb(name, shape, dt):
                    return les.enter_context(nc.sbuf_tensor(un(name), shape, dt))
                half = (len(tiles) + 1) // 2
                groups = [tiles[:half], tiles[half:]]
                ng = max(len(g) for g in groups)
                acc = sb("eacc", [128, ng, D], F32)
                h2g = sb("eh2", [128, 16, ng * 128], BF16)
                h2gb = Buf()
                les2 = contextlib.ExitStack()
                les2.__enter__()
                sb_outer = sb

                def sb(name, shape, dt):
                    return les2.enter_context(nc.sbuf_tensor(un(name), shape, dt))
                accb = [bufs(4) for _ in range(ng)]
                heT = sb("ehe", [128, 8, ng * 128], BF16)
                heb = Buf()
                wg_s = [(sb("ewg%d" % i, [128, 16, 128], BF16), Buf()) for i in range(2)]
                wu_s = [(sb("ewu%d" % i, [128, 16, 128], BF16), Buf()) for i in range(2)]
                wd_s = [(sb("ewd%d" % i, [128, 8, 512], BF16), Buf()) for i in range(2)]
                sgr = Rot([(sb("esg%d" % i, [128, 512], F32), Buf()) for i in range(2)])
                gb = Buf()
                for gi_, grp in enumerate(groups):
                    t0 = grp[0] * 128
                    ntok = len(grp) * 128
                    if gi_ > 0:
                        les2 = contextlib.ExitStack()
                        les2.__enter__()
                        wg_s = [(sb("ewg%d" % i, [128, 16, 128], BF16), Buf()) for i in range(2)]
                        wu_s = [(sb("ewu%d" % i, [128, 16, 128], BF16), Buf()) for i in range(2)]
                        wd_s = [(sb("ewd%d" % i, [128, 8, 512], BF16), Buf()) for i in range(2)]
                        heT = sb("ehe", [128, 8, ng * 128], BF16)
                        sgr = Rot([(sb("esg%d" % i, [128, 512], F32), Buf()) for i in range(2)])
                    k.dma(SY, h2g[:, :, 0:ntok], H2T.rearrange("(c p) t -> p c t", p=128)[:, :, t0:t0 + ntok], reads=[fb2], writes=[h2gb])
                    if ntok == 1152:
                        subs = [(0, 384), (384, 384), (768, 384)]
                    else:
                        subs = [(s_, min(512, ntok - s_)) for s_ in range(0, ntok, 512)]

                    def ldg(i, slot):
                        k.dma(GQ, slot[0][:].rearrange("p a b -> p (a b)"), weg_in[l, i // 8, i % 8], writes=[slot[1]])

                    def ldu(i, slot):
                        k.dma(GQ, slot[0][:].rearrange("p a b -> p (a b)"), weu_in[l, i // 8, i % 8], writes=[slot[1]])

                    def ldd(i, slot):
                        k.dma(GQ, slot[0][:].rearrange("p a b -> p (a b)"), wed_in[l, i // 4, i % 4], writes=[slot[1]])
                    sg_ = WStream(wg_s, ldg, NE * 8)
                    su_ = WStream(wu_s, ldu, NE * 8)
                    sd_ = WStream(wd_s, ldd, NE * 4)
                    for e_ in range(NE):
                        for j in range(8):
                            wg, wgb = sg_.get(e_ * 8 + j)
                            wu, wub = su_.get(e_ * 8 + j)
                            for (s0, sn) in subs:
                                pg, pgb = mmr.next()
                                pu, pub = mmr.next()
                                rb = h2gb
                                for kc in range(16):
                                    k.op(PE, lambda e: e.matmul(pg[:, 0:sn], wg[:, kc, :], h2g[:, kc, s0:s0 + sn], start=(kc == 0), stop=(kc == 15)),
                                         reads=[wgb, rb], writes=[pgb])
                                for kc in range(16):
                                    k.op(PE, lambda e: e.matmul(pu[:, 0:sn], wu[:, kc, :], h2g[:, kc, s0:s0 + sn], start=(kc == 0), stop=(kc == 15)),
                                         reads=[wub, rb], writes=[pub])
                                sg, sgb = sgr.next()
                                k.op(ACT, lambda e: e.activation(out=sg[:, 0:sn], in_=pg[:, 0:sn], func=AF.Silu), reads=[pgb], writes=[sgb])
                                k.op(DVE, lambda e: e.tensor_tensor(out=heT[:, j, s0:s0 + sn], in0=pu[:, 0:sn], in1=sg[:, 0:sn], op=ALU.mult),
                                     reads=[pub, sgb], writes=[heb])
                        for n in range(4):
                            wd, wdb = sd_.get(e_ * 4 + n)
                            for mi, m in enumerate(grp):
                                pt, ptb = mmr.next()
                                for jc in range(8):
                                    k.op(PE, lambda e: e.matmul(pt[:, :], heT[:, jc, mi * 128:(mi + 1) * 128], wd[:, jc, :], start=(jc == 0), stop=(jc == 7)),
                                         reads=[wdb, heb], writes=[ptb])
                                dst = acc[:, mi, n * 512:(n + 1) * 512]
                                if e_ == 0:
                                    k.op(DVE, lambda e: e.tensor_scalar(out=dst, in0=pt[:, :], scalar1=comb[:, m, e_:e_ + 1], scalar2=None, op0=ALU.mult),
                                         reads=[ptb, combb[m]], writes=[accb[mi][n]])
                                else:
                                    k.op(DVE, lambda e: e.scalar_tensor_tensor(out=dst, in0=pt[:, :], scalar=comb[:, m, e_:e_ + 1], in1=dst, op0=ALU.mult, op1=ALU.add),
                                         reads=[ptb, combb[m]], writes=[accb[mi][n]])
                    k.barrier()
                    les2.__exit__(None, None, None)
                    les3 = contextlib.ExitStack()
                    les3.__enter__()

                    def sb3(name, shape, dt):
                        return les3.enter_context(nc.sbuf_tensor(un(name), shape, dt))
                    gt2 = sb3("egt", [128, 2, D], F32)
                    gfin = sb3("egf", [128, D], F32)
                    k.xt_r = Rot([(sb3("ext%d" % i, [128, D], F32), Buf()) for i in range(3)])
                    k.xn_r = Rot([(sb3("exn%d" % i, [128, D], F32), Buf()) for i in range(1)])
                    for v in range(2):
                        k.dma(SY, gt2[:, v, :], MODROW[l, v, 80 * 128:96 * 128].partition_broadcast(128), reads=[modb], writes=[gb])
                    k.dma(SY, gfin[:], gfin_in.partition_broadcast(128), writes=[gb])
                    rld = {}

                    def issue_r(m_):
                        xt_, xtb_ = k.xt_r.next()
                        k.dma(SY, xt_[:], XL[m_ * 128:(m_ + 1) * 128, :], reads=XB[m_], writes=[xtb_])
                        rld[m_] = (xt_, xtb_)
                    issue_r(grp[0])
                    for mi, m in enumerate(grp):
                        v = 0 if m < 16 else 1
                        if mi + 1 < len(grp):
                            issue_r(grp[mi + 1])
                        xt, xtb = rld.pop(m)
                        k.op(DVE, lambda e: e.tensor_tensor(out=acc[:, mi, :], in0=acc[:, mi, :], in1=gt2[:, v, :], op=ALU.mult), reads=[accb[mi], gb], writes=[accb[mi]])
                        k.op(DVE, lambda e: e.tensor_tensor(out=xt[:], in0=xt[:], in1=acc[:, mi, :], op=ALU.add), reads=[xtb, accb[mi]], writes=[xtb])
                        if not last:
                            k.dma(SY, XL[m * 128:(m + 1) * 128, :], xt[:], reads=[xtb], writes=XB[m])
                        else:
                            xn, xnb = k.xn_r.next()
                            ss, ssb = sc_r.next()
                            k.op(DVE, lambda e: e.memset(ss[:, 0:1], 0.0), writes=[ssb])
                            k.op(ACT, lambda e: e.activation(out=xn[:], in_=xt[:], func=AF.Square, accum_out=ss[:, 0:1]), reads=[xtb], writes=[xnb, ssb])
                            rstd_of(ss[:, 0:1], ssb, D)
                            k.op(DVE, lambda e: e.scalar_tensor_tensor(out=xn[:], in0=xt[:], scalar=ss[:, 0:1], in1=gfin[:], op0=ALU.mult, op1=ALU.mult),
                                 reads=[xtb, ssb, gb], writes=[xnb])
                            k.dma(SY, y_out[m * 128:(m + 1) * 128, :], xn[:], reads=[xnb], writes=[])
                    k.barrier()
                    les3.__exit__(None, None, None)

        k.rt_r = Rot([(k.sb("rtR%d" % i, [128, 160], F32), Buf()) for i in range(2)])
        ALLT = list(range(NTILE))
        LAT = list(range(16))
        for l in range(n_layers):
            last = (l == L - 1)
            k.barrier()
            with Scope() as S:
                alloc_big(S)
                alloc_wslot(S)
                alloc_st(S)
                with Scope() as S2:
                    alloc_x(S2)
                    phase_norm(l, 1, ALLT)
                    if "HD" in dbg and l == 0:
                        k.dma(SY, HD.rearrange("(c p) t -> p c t", p=128), k.BIG[:], reads=BIGB, writes=[])
                    k.barrier()
                if stop == "norm1":
                    break
                gi = phase_inproj(l, S, last)
                for tok_ in gi:
                    if tok_ == "AUAV":
                        break
                fb_a = fence_bufs()
                with Scope() as S3:
                    gs_ = phase_sgu(l, fb_a, LAT if last else ALLT, S3.sb, GQ)
                    live = [True, True]
                    cnt_ = 0
                    while live[0] or live[1]:
                        if live[0]:
                            try:
                                next(gi)
                            except StopIteration:
                                live[0] = False
                        cnt_ += 1
                        if live[1] and (cnt_ % 3 == 0 or not live[0]):
                            try:
                                next(gs_)
                            except StopIteration:
                                live[1] = False
                    k.barrier()
            fb = fence_bufs()
            if stop in ("inproj", "sgu"):
                break
            k.barrier()
            phase_rope(fb)
            fb = fence_bufs()
            if stop == "na":
                break
            k.barrier()
            with Scope() as S:
                gens = [phase_na(l, fb, not last, S.sb), phase_mlstm(l, fb, not last, S.sb)]
                while gens:
                    for g_ in list(gens):
                        try:
                            next(g_)
                        except StopIteration:
                            gens.remove(g_)
                k.barrier()
            fb = fence_bufs()
            if stop == "mlstm":
                break
            k.barrier()
            phase_merge(l, fb, TG[:4] if last else TG)
            fb = fence_bufs()
            if stop == "merge":
                break
            k.barrier()
            with Scope() as S:
                alloc_big(S)
                alloc_wslot(S)
                phase_out(l, fb, LAT if last else ALLT)
            if stop == "out":
                break
            k.barrier()
            with Scope() as S:
                alloc_x(S)
                k.hf_r = Rot([(S.sb("hf%d" % i, [128, 16, 128], F32), bufs(16)) for i in range(2)])
                k.h2s_r = Rot([(S.sb("h2s%d" % i, [128, 16, 128], BF16), bufs(16)) for i in range(2)])
                phase_norm(l, 2, LAT if last else ALLT, router=True)
            fb2 = fence_bufs()
            if stop == "norm2":
                break
            k.barrier()
            phase_moe(l, LAT if last else ALLT, last, fb2)
        k.barrier()
    return nc


def _c(a):
    return np.ascontiguousarray(a, dtype=np.float32)


def prep_shared(inp):
    global _NA_IDX
    W = {}
    w_ada = inp["w_ada"]
    W["wada"] = _c(w_ada.reshape(L, 16, 128, 24, 512).transpose(0, 3, 2, 1, 4)).reshape(L, 24, 128, 16 * 512)
    W["bada"] = _c(inp["b_ada"].reshape(L, 96, 128).transpose(2, 0, 1))
    W["g1"] = _c(inp["g_norm1"].reshape(L, 16, 128).transpose(2, 0, 1))
    W["g2"] = _c(inp["g_norm2"].reshape(L, 16, 128).transpose(2, 0, 1))
    w_in = inp["w_in"]
    W["win"] = _c(w_in[:, :, 0:8192].reshape(L, 16, 128, 16, 512).transpose(0, 3, 2, 1, 4)).reshape(L, 16, 128, 16 * 512)
    W["wcg"] = _c(w_in[:, :, 8192:8208].reshape(L, 16, 128, 16).transpose(0, 2, 1, 3)).reshape(L, 128, 256)
    W["wgate"] = _c(w_in[:, :, 8208:14352].reshape(L, 16, 128, 12, 512).transpose(0, 3, 2, 1, 4)).reshape(L, 12, 128, 16 * 512)
    W["gsgu"] = _c(inp["g_sgu"])
    W["wsp"] = _c(inp["w_spatial"].transpose(0, 3, 1, 2)).reshape(L, 128, 1024)
    W["bsp"] = _c(inp["b_spatial"].reshape(L, 1024))
    if _NA_IDX is None:
        _NA_IDX = na_index_tables()
    rpb = inp["na_rpb"].reshape(L, 8, 465)
    rpbx = np.concatenate([rpb, np.full((L, 8, 1), -1e30, np.float32)], axis=2)
    tab = rpbx[:, :, _NA_IDX]
    W["natab"] = _c(tab.transpose(0, 1, 3, 2, 4, 5)).reshape(L, 8, 128, 5 * 5 * 128)
    t = np.arange(SEQ)
    pr, pc = (t // 64).astype(np.float64), (t % 64).astype(np.float64)
    inv = 10000.0 ** (-np.arange(32, dtype=np.float64) / 32)
    d = np.arange(128)
    pos = np.where(d[:, None] < 64, pr[None, :], pc[None, :])
    ang = pos * inv[d % 32][:, None]
    cos = np.cos(ang)
    sins = np.sin(ang) * np.where((d % 64) < 32, -1.0, 1.0)[:, None]
    sc = 128 ** -0.5
    W["rope"] = _c(np.stack([cos * sc, sins * sc, cos, sins], axis=1)).reshape(128, 4 * SEQ)
    ident = np.eye(128)
    s_i, t_i = np.meshgrid(np.arange(128), np.arange(128), indexing="ij")
    trif = (s_i <= t_i).astype(np.float64)
    trib = (s_i >= t_i).astype(np.float64)
    perm = np.zeros((128, 128))
    for m in range(128):
        partner = m + 32 if (m % 64) < 32 else m - 32
        perm[partner, m] = 1.0
    W["consts"] = _c(np.stack([ident, trif, trib, np.ones((128, 128)), perm], axis=1)).reshape(128, 5 * 128)
    W["bmg"] = _c(inp["b_mgate"].reshape(L, 16))
    W["gmn"] = _c(inp["g_mnorm"])
    W["wbr"] = _c(inp["w_branch"].reshape(L, 3, 8, 128, 16, 128).transpose(0, 4, 3, 1, 2, 5)).reshape(L, 16, 128, 3 * 8 * 128)
    W["wout"] = _c(inp["w_out"].reshape(L, 16, 128, 4, 512).transpose(0, 3, 2, 1, 4)).reshape(L, 4, 128, 16 * 512)
    W["wr"] = _c(inp["w_router"].reshape(16, 128, 16).transpose(1, 0, 2)).reshape(128, 256)
    W["br"] = _c(inp["b_router"])
    W["weg"] = _c(inp["w_e_gate"].reshape(L, NE, 16, 128, 8, 128).transpose(0, 1, 4, 3, 2, 5)).reshape(L, NE, 8, 128, 16 * 128)
    W["weu"] = _c(inp["w_e_up"].reshape(L, NE, 16, 128, 8, 128).transpose(0, 1, 4, 3, 2, 5)).reshape(L, NE, 8, 128, 16 * 128)
    W["wed"] = _c(inp["w_e_down"].reshape(L, NE, 8, 128, 4, 512).transpose(0, 1, 4, 3, 2, 5)).reshape(L, NE, 4, 128, 8 * 512)
    W["gfin"] = _c(inp["g_final"])
    return W


def prep_core(inp, b):
    m = {}
    m["x"] = _c(inp["x"][b])
    m["ctx"] = _c(inp["ctx"][b])
    cv = np.stack([inp["c"][b].reshape(16, 128), inp["c_ctx"].reshape(16, 128)], axis=-1)
    m["cvec"] = _c(cv.transpose(1, 0, 2))
    return m


def kernel(**inputs):
    inp = {k_: np.asarray(v) for k_, v in inputs.items()}
    W = prep_shared(inp)
    nc = build()
    n = 8
    in_maps = []
    for b in range(n):
        m = dict(W)
        m.update(prep_core(inp, b))
        in_maps.append(m)
    res = run_bass_kernel_spmd(nc, in_maps, core_ids=list(range(n)))
    return np.stack([np.asarray(r["y"], dtype=np.float32) for r in res.results], axis=0)
```
